# Optimizing a Trainium2 kernel written in Bass

```python
import jax, jax.numpy as jnp
from jax import lax
import numpy as np

D_MODEL = 1024
BATCH = 8
SEQ = 4096
DEPTH = 2

GRID_W = 64
CTX_LEN = 256
EPS = 1e-6
HEAD_DIM = 64
ATTN_W = D_MODEL // 2
N_Q_HEADS = ATTN_W // HEAD_DIM
N_KV_HEADS = N_Q_HEADS // 4
Q_GROUP = N_Q_HEADS // N_KV_HEADS
KV_W = N_KV_HEADS * HEAD_DIM
Q_BLOCK = 128
ROPE_THETA = 10000.0
AXIS_DIM = HEAD_DIM // 2
CONV_W = D_MODEL // 4
CONV_K = 31
CHUNK_W = D_MODEL // 4
CHUNK_HEADS = 4
CHUNK_HEAD_DIM = CHUNK_W // CHUNK_HEADS
CHUNK = 128
MIX_W = ATTN_W + CONV_W + CHUNK_W
Q0 = 0
K0 = Q0 + ATTN_W
V0 = K0 + KV_W
CV0 = V0 + KV_W
CH0 = CV0 + 2 * CONV_W
IN_W = CH0 + 2 * CHUNK_W
N_EXPERTS = 32
TOP_K = 4
D_EXPERT = D_MODEL
SWIGLU_LIMIT = 7.0
SWIGLU_ALPHA = 1.702
EXPERT_BLOCK = 128

kernel_name = "hybrid_parallel_groups_moe_dit"


def rmsnorm(x, g):
    xf = x.astype(jnp.float32)
    y = xf * lax.rsqrt(jnp.mean(xf * xf, axis=-1, keepdims=True) + EPS)
    return (y * g.astype(jnp.float32)).astype(x.dtype)


def layernorm(x, g, b):
    xf = x.astype(jnp.float32)
    mu = jnp.mean(xf, axis=-1, keepdims=True)
    xc = xf - mu
    y = xc * lax.rsqrt(jnp.mean(xc * xc, axis=-1, keepdims=True) + EPS)
    return (y * g.astype(jnp.float32) + b.astype(jnp.float32)).astype(x.dtype)


def axial_rope_tables(n_tokens):
    rows = n_tokens // GRID_W
    r, col = jnp.meshgrid(jnp.arange(rows), jnp.arange(GRID_W), indexing="ij")
    inv = ROPE_THETA ** (-jnp.arange(0, AXIS_DIM, 2, dtype=jnp.float32) / AXIS_DIM)
    ang = jnp.concatenate([r.reshape(-1, 1).astype(jnp.float32) * inv,
                           col.reshape(-1, 1).astype(jnp.float32) * inv], axis=-1)
    return jnp.cos(ang), jnp.sin(ang)


def apply_rope(t, cos, sin):
    tf = t.astype(jnp.float32)
    half = HEAD_DIM // 2
    t1, t2 = tf[..., :half], tf[..., half:]
    cs, sn = cos[None, :, None, :], sin[None, :, None, :]
    return jnp.concatenate([t1 * cs - t2 * sn, t2 * cs + t1 * sn], axis=-1).astype(t.dtype)


def attend(q, k, v):
    s = jnp.einsum("bqkgd,bskd->bkgqs", q, k, preferred_element_type=jnp.float32) * (HEAD_DIM ** -0.5)
    p = jax.nn.softmax(s, axis=-1).astype(v.dtype)
    return jnp.einsum("bkgqs,bskd->bqkgd", p, v)


def latent_attention(q, k_all, v_all):
    B, S = q.shape[0], q.shape[1]
    nb = S // Q_BLOCK
    qb = q.reshape(B, nb, Q_BLOCK, N_KV_HEADS, Q_GROUP, HEAD_DIM).transpose(1, 0, 2, 3, 4, 5)
    o = lax.map(lambda qi: attend(qi, k_all, v_all), qb)
    return o.transpose(1, 0, 2, 3, 4, 5).reshape(B, S, ATTN_W)


def conformer_conv(z, w_dw, b_dw, g_ln, b_ln):
    u = z[..., :CONV_W] * jax.nn.sigmoid(z[..., CONV_W:])
    u = lax.conv_general_dilated(u, w_dw[:, None, :], window_strides=(1,),
                                 padding=((CONV_K // 2, CONV_K // 2),),
                                 dimension_numbers=("NWC", "WIO", "NWC"),
                                 feature_group_count=CONV_W) + b_dw
    return jax.nn.silu(layernorm(u, g_ln, b_ln))


def chunk_spatial_gating(z, g_ln, b_ln, w_s, b_s):
    z = jax.nn.gelu(z, approximate=False)
    u, v = z[..., :CHUNK_W], z[..., CHUNK_W:]
    v = layernorm(v, g_ln, b_ln)
    B, L = v.shape[0], v.shape[1]
    vc = v.reshape(B, L // CHUNK, CHUNK, CHUNK_HEADS, CHUNK_HEAD_DIM)
    s = jnp.einsum("hpq,bnqhd->bnphd", w_s, vc) + b_s.T[None, None, :, :, None]
    return u * s.reshape(B, L, CHUNK_W)


def token_mixer(h_lat, h_ctx, w_in, g_q, g_k, w_dw, b_dw, g_cln, b_cln, g_sln, b_sln, w_s, b_s, w_o,
                cos, sin, ctx_out):
    B, S = h_lat.shape[0], h_lat.shape[1]
    C = h_ctx.shape[1]
    p = h_lat @ w_in
    q = apply_rope(rmsnorm(p[..., Q0:K0].reshape(B, S, N_Q_HEADS, HEAD_DIM), g_q), cos, sin)
    k = apply_rope(rmsnorm(p[..., K0:V0].reshape(B, S, N_KV_HEADS, HEAD_DIM), g_k), cos, sin)
    v = p[..., V0:CV0].reshape(B, S, N_KV_HEADS, HEAD_DIM)
    if ctx_out:
        pc = h_ctx @ w_in
        kvc = pc[..., K0:CV0]
    else:
        kvc = h_ctx @ w_in[:, K0:CV0]
    kc = rmsnorm(kvc[..., :KV_W].reshape(B, C, N_KV_HEADS, HEAD_DIM), g_k)
    vc = kvc[..., KV_W:].reshape(B, C, N_KV_HEADS, HEAD_DIM)
    k_all = jnp.concatenate([k, kc], axis=1)
    v_all = jnp.concatenate([v, vc], axis=1)
    a_lat = latent_attention(q, k_all, v_all)
    conv_lat = conformer_conv(p[..., CV0:CH0], w_dw, b_dw, g_cln, b_cln)
    chunk_lat = chunk_spatial_gating(p[..., CH0:IN_W], g_sln, b_sln, w_s, b_s)
    y_lat = jnp.concatenate([a_lat, conv_lat, chunk_lat], axis=-1) @ w_o
    if not ctx_out:
        return y_lat, None
    qc = rmsnorm(pc[..., Q0:K0].reshape(B, C, N_Q_HEADS, HEAD_DIM), g_q)
    a_ctx = attend(qc.reshape(B, C, N_KV_HEADS, Q_GROUP, HEAD_DIM), kc, vc).reshape(B, C, ATTN_W)
    conv_ctx = conformer_conv(pc[..., CV0:CH0], w_dw, b_dw, g_cln, b_cln)
    chunk_ctx = chunk_spatial_gating(pc[..., CH0:IN_W], g_sln, b_sln, w_s, b_s)
    y_ctx = jnp.concatenate([a_ctx, conv_ctx, chunk_ctx], axis=-1) @ w_o
    return y_lat, y_ctx


def moe_ffn(h, w_router, b_router, w_gate, b_gate, w_up, b_up, w_down, b_down):
    N, D = h.shape
    logits = (h @ w_router).astype(jnp.float32) + b_router.astype(jnp.float32)
    top_vals, top_idx = lax.top_k(logits, TOP_K)
    weights = jax.nn.softmax(top_vals, axis=-1)
    A = N * TOP_K
    flat_e = top_idx.reshape(-1)
    flat_tok = jnp.repeat(jnp.arange(N, dtype=jnp.int32), TOP_K)
    flat_w = weights.reshape(-1)
    order = jnp.argsort(flat_e)
    e_sorted = flat_e[order]
    counts = jnp.bincount(flat_e, length=N_EXPERTS)
    padded = (counts + EXPERT_BLOCK - 1) // EXPERT_BLOCK * EXPERT_BLOCK
    ends = jnp.cumsum(counts)
    pends = jnp.cumsum(padded)
    dest = (pends - padded)[e_sorted] + jnp.arange(A, dtype=jnp.int32) - (ends - counts)[e_sorted]
    n_blocks = -(-A // EXPERT_BLOCK) + N_EXPERTS
    P = n_blocks * EXPERT_BLOCK
    buf_tok = jnp.zeros((P,), jnp.int32).at[dest].set(flat_tok[order])
    buf_w = jnp.zeros((P,), jnp.float32).at[dest].set(flat_w[order])
    block_e = jnp.minimum(jnp.searchsorted(pends, jnp.arange(n_blocks, dtype=jnp.int32) * EXPERT_BLOCK,
                                           side="right"), N_EXPERTS - 1)

    def expert_block(args):
        tok, e = args
        xb = h[tok]
        g = jnp.minimum(xb @ w_gate[e] + b_gate[e], SWIGLU_LIMIT)
        u = jnp.clip(xb @ w_up[e] + b_up[e], -SWIGLU_LIMIT, SWIGLU_LIMIT)
        a = (u + 1.0) * (g * jax.nn.sigmoid(SWIGLU_ALPHA * g))
        return a @ w_down[e] + b_down[e]

    ys = lax.map(expert_block, (buf_tok.reshape(n_blocks, EXPERT_BLOCK), block_e))
    out = jnp.zeros((N, D), jnp.float32).at[buf_tok].add(ys.reshape(P, D).astype(jnp.float32) * buf_w[:, None])
    return out.astype(h.dtype)


def setup_inputs(seed: int = 0) -> dict:
    key = jax.random.key(seed)
    ks = jax.random.split(key, 40)
    f32 = jnp.float32

    def nrm(k, shape, scale):
        return jax.random.normal(k, shape, f32) * scale

    L, D, E, F = DEPTH, D_MODEL, N_EXPERTS, D_EXPERT
    return {
        "x": nrm(ks[0], (BATCH, SEQ, D), 1.0),
        "c": nrm(ks[1], (BATCH, D), 1.0),
        "ctx": nrm(ks[2], (BATCH, CTX_LEN, D), 1.0),
        "c_ctx": nrm(ks[3], (D,), 1.0),
        "w_ada": nrm(ks[4], (L, D, 6 * D), 0.5 * D ** -0.5),
        "b_ada": nrm(ks[5], (L, 6 * D), 0.02),
        "g_norm1": 1.0 + nrm(ks[6], (L, D), 0.02),
        "w_in": nrm(ks[7], (L, D, IN_W), D ** -0.5),
        "g_q": 1.0 + nrm(ks[8], (L, HEAD_DIM), 0.02),
        "g_k": 1.0 + nrm(ks[9], (L, HEAD_DIM), 0.02),
        "w_dw": nrm(ks[10], (L, CONV_K, CONV_W), CONV_K ** -0.5),
        "b_dw": nrm(ks[11], (L, CONV_W), 0.02),
        "g_conv_ln": 1.0 + nrm(ks[12], (L, CONV_W), 0.02),
        "b_conv_ln": nrm(ks[13], (L, CONV_W), 0.02),
        "g_sgu_ln": 1.0 + nrm(ks[14], (L, CHUNK_W), 0.02),
        "b_sgu_ln": nrm(ks[15], (L, CHUNK_W), 0.02),
        "w_s": nrm(ks[16], (L, CHUNK_HEADS, CHUNK, CHUNK), CHUNK ** -0.5),
        "b_s": nrm(ks[17], (L, CHUNK_HEADS, CHUNK), 0.02),
        "w_o": nrm(ks[18], (L, MIX_W, D), MIX_W ** -0.5),
        "g_norm2": 1.0 + nrm(ks[19], (L, D), 0.02),
        "w_router": nrm(ks[20], (L, D, E), D ** -0.5),
        "b_router": nrm(ks[21], (L, E), 0.01),
        "w_gate": nrm(ks[22], (L, E, D, F), D ** -0.5),
        "b_gate": nrm(ks[23], (L, E, F), 0.02),
        "w_up": nrm(ks[24], (L, E, D, F), D ** -0.5),
        "b_up": nrm(ks[25], (L, E, F), 0.02),
        "w_down": nrm(ks[26], (L, E, F, D), F ** -0.5),
        "b_down": nrm(ks[27], (L, E, D), 0.02),
        "g_final": 1.0 + nrm(ks[28], (D,), 0.02),
    }


def reference(x, c, ctx, c_ctx, w_ada, b_ada, g_norm1, w_in, g_q, g_k, w_dw, b_dw, g_conv_ln, b_conv_ln,
              g_sgu_ln, b_sgu_ln, w_s, b_s, w_o, g_norm2, w_router, b_router, w_gate, b_gate, w_up, b_up,
              w_down, b_down, g_final):
    B, S, D = x.shape
    C = ctx.shape[1]
    cos, sin = axial_rope_tables(S)
    xc = ctx
    sc_lat = jax.nn.silu(c)
    sc_ctx = jax.nn.silu(c_ctx)
    for l in range(DEPTH):
        last = l == DEPTH - 1
        mod_l = sc_lat @ w_ada[l] + b_ada[l]
        mod_c = sc_ctx @ w_ada[l] + b_ada[l]
        sh1, sc1, gt1, sh2, sc2, gt2 = jnp.split(mod_l[:, None, :], 6, axis=-1)
        csh1, csc1, cgt1, csh2, csc2, cgt2 = jnp.split(mod_c, 6, axis=-1)
        h_lat = rmsnorm(x, g_norm1[l]) * (1.0 + sc1) + sh1
        h_ctx = rmsnorm(xc, g_norm1[l]) * (1.0 + csc1) + csh1
        y_lat, y_ctx = token_mixer(h_lat, h_ctx, w_in[l], g_q[l], g_k[l], w_dw[l], b_dw[l],
                                   g_conv_ln[l], b_conv_ln[l], g_sgu_ln[l], b_sgu_ln[l], w_s[l], b_s[l],
                                   w_o[l], cos, sin, not last)
        x = x + gt1 * y_lat
        h2_lat = rmsnorm(x, g_norm2[l]) * (1.0 + sc2) + sh2
        if not last:
            xc = xc + cgt1 * y_ctx
            h2_ctx = rmsnorm(xc, g_norm2[l]) * (1.0 + csc2) + csh2
            tokens = jnp.concatenate([h2_lat.reshape(B * S, D), h2_ctx.reshape(B * C, D)], axis=0)
            m = moe_ffn(tokens, w_router[l], b_router[l], w_gate[l], b_gate[l], w_up[l], b_up[l],
                        w_down[l], b_down[l])
            x = x + gt2 * m[:B * S].reshape(B, S, D)
            xc = xc + cgt2 * m[B * S:].reshape(B, C, D)
        else:
            m = moe_ffn(h2_lat.reshape(B * S, D), w_router[l], b_router[l], w_gate[l], b_gate[l],
                        w_up[l], b_up[l], w_down[l], b_down[l])
            x = x + gt2 * m.reshape(B, S, D)
    return rmsnorm(x, g_final)
```

```python
import numpy as np
from contextlib import ExitStack
import concourse.bass as bass
import concourse.mybir as mybir
from concourse.bass_utils import run_bass_kernel_spmd

F32 = mybir.dt.float32
BF16 = mybir.dt.bfloat16
I32 = mybir.dt.int32
U32 = mybir.dt.uint32
AF = mybir.ActivationFunctionType
ALU = mybir.AluOpType
AX = mybir.AxisListType

S = 4096
C = 256
NTOK = S + C
D = 1024
KD = 8
INW = 1792
NE = 32
CAP = 2048
NSLOT = NE * CAP
EPS = 1e-6
BIG = 4.0e6


class Buf:
    __slots__ = ("name", "w", "r")

    def __init__(self, name=""):
        self.name = name
        self.w = {}
        self.r = {}


class Eng:
    def __init__(self, name, h, sem):
        self.name, self.h, self.sem = name, h, sem
        self.count = 0
        self.waited = {}


class FW:
    def __init__(self, nc, stack):
        self.nc = nc
        self.stack = stack
        self.nsem = 0
        self.pe = self._eng("pe", nc.tensor)
        self.act = self._eng("act", nc.scalar)
        self.dve = self._eng("dve", nc.vector)
        self.pool = self._eng("pool", nc.gpsimd)
        self.sp = self._eng("sp", nc.sync)
        self.engs = [self.pe, self.act, self.dve, self.pool, self.sp]
        self.dma_sems = {}
        self.all_dma = []

    def new_sem(self, name):
        self.nsem += 1
        return self.stack.enter_context(self.nc.semaphore(name))

    def _eng(self, name, h):
        return Eng(name, h, self.new_sem("s_" + name))

    def _wait(self, eng, sem, val):
        key = sem.num
        if eng.waited.get(key, 0) >= val:
            return
        eng.waited[key] = val
        eng.h.wait_ge(sem, val)

    def _deps(self, eng, reads, writes, waw=True):
        for b in reads:
            for s, v in b.w.values():
                if s is eng.sem and eng is self.pe:
                    continue
                self._wait(eng, s, v)
        for b in writes:
            for d in ((b.w, b.r) if waw else (b.r,)):
                for s, v in d.values():
                    if s is eng.sem and eng is self.pe:
                        continue
                    self._wait(eng, s, v)

    @staticmethod
    def _rec(d, sem, val):
        k = sem.num
        if k not in d or d[k][1] < val:
            d[k] = (sem, val)

    def _mark(self, sem, val, reads, writes):
        for b in writes:
            b.r = {}
            self._rec(b.w, sem, val)
        for b in reads:
            self._rec(b.r, sem, val)

    def op(self, eng, fn, reads=(), writes=(), inc=True):
        self._deps(eng, reads, writes)
        ins = fn()
        if inc:
            eng.count += 1
            ins.then_inc(eng.sem, 1)
            self._mark(eng.sem, eng.count, reads, writes)
        else:
            self._mark(eng.sem, eng.count + 1, reads, writes)
        return ins

    def dma(self, q, fn, reads=(), writes=(), owner=None, waw=True):
        self._deps(q, reads, writes, waw=waw)
        owner = owner or (writes[0] if writes else reads[0])
        ent = self.dma_sems.get(id(owner))
        if ent is None:
            ent = [self.new_sem("d%d" % self.nsem), 0, owner]
            self.dma_sems[id(owner)] = ent
            self.all_dma.append(ent)
        ent[1] += 16
        ins = fn()
        ins.then_inc(ent[0], 16)
        for b in writes:
            if waw:
                b.r = {}
            self._rec(b.w, ent[0], ent[1])
        for b in reads:
            self._rec(b.r, ent[0], ent[1])
        return ins

    def share(self, owner, *others):
        ent = self.dma_sems.get(id(owner))
        if ent is None:
            ent = [self.new_sem("d%d" % self.nsem), 0, owner]
            self.dma_sems[id(owner)] = ent
            self.all_dma.append(ent)
        for o in others:
            self.dma_sems[id(o)] = ent

    def barrier(self):
        for e in self.engs:
            for x in self.engs:
                if x is not e and x.count > 0:
                    self._wait(e, x.sem, x.count)
            for ent in self.all_dma:
                if ent[1] > 0:
                    self._wait(e, ent[0], ent[1])


def build(nlayers=2, dbg=False, stop_after=None):
    nc = bass.Bass("TRN2", target_bir_lowering=False)

    def din(name, shape, dt=F32):
        return nc.dram_tensor(name, list(shape), dt, kind="ExternalInput").ap()

    def dscr(name, shape, dt, big=False):
        return nc.dram_tensor(name, list(shape), dt, kind="ExternalOutput" if (dbg and not big) else "Internal").ap()

    x_in = din("x", [S, D]); ctx_in = din("ctx", [C, D])
    cc_in = din("cc", [128, KD, 2])
    wada_in = din("w_ada", [2, D, 6 * D]); badac_in = din("b_adac", [128, 2, 48]); bada_in = din("b_ada", [2, 6 * D])
    g1c_in = din("g1c", [128, 2, KD]); g2bc_in = din("g2bc", [2, 128, D]); gfbc_in = din("gfbc", [128, D])
    win_in = din("w_in", [2, D, INW]); wo_in = din("w_o", [2, D, D]); wr_in = din("w_router", [2, D, NE])
    brbc_in = din("b_rbc", [128, 2, NE]); gqk_in = din("gqk", [128, 2, 640]); cs_in = din("cs", [128, 32, 64])
    wdwc_in = din("wdwc", [128, 2, 2, 31]); cvp_in = din("cvp", [128, 2, 2, 3])
    sgp_in = din("sgp", [128, 2, 2, 256]); wsT_in = din("wsT", [128, 2, 4, 128]); bsc_in = din("bsc", [128, 2, 4])
    wg_in = din("w_gate", [2, NE, D, D]); wu_in = din("w_up", [2, NE, D, D]); wd_in = din("w_down", [2, NE, D, D])
    bgu_in = din("bgu", [128, 2, NE, 2, KD]); bd_in = din("b_down", [2, NE, D])
    ident_in = din("ident", [128, 128]); tri_in = din("tri", [128, 128]); eoff_in = din("eoff", [128, NE])
    iota_in = din("iota", [128, NE])
    out_d = nc.dram_tensor("out", [S, D], F32, kind="ExternalOutput").ap()

    QS = dscr("QS", [NTOK, 512], BF16); KS = dscr("KS", [NTOK, 128], BF16)
    US = dscr("US", [16 + S + 32, 256], BF16); USC = dscr("USC", [16 + C + 32, 256], BF16)
    CS = dscr("CS", [NTOK, 256], BF16)
    XM = dscr("XM", [NTOK, D], F32); XS = dscr("XS", [NTOK, D], F32)
    XG = dscr("XG", [NSLOT, D], BF16, big=True); YG = dscr("YG", [NSLOT, D], F32, big=True)
    bQS, bKS, bUS, bUSC, bCS, bXM, bXS, bXG, bYG, bOUT = [Buf(n) for n in
                                                          "QS KS US USC CS XM XS XG YG OUT".split()]
    bIN = Buf("inputs")

    with ExitStack() as top:
        fw = FW(nc, top)
        uid = [0]

        def T(stack, shape, dt, name=None):
            uid[0] += 1
            return stack.enter_context(nc.sbuf_tensor(name or "t%d" % uid[0], list(shape), dt))

        def mk(eng, h):
            def f(name, R=(), W=(), inc=True, **kw):
                return fw.op(eng, lambda: getattr(h, name)(**kw), R, W, inc)
            return f
        dve = mk(fw.dve, nc.vector); act = mk(fw.act, nc.scalar); pool = mk(fw.pool, nc.gpsimd)

        def mm(out, lhsT, rhs, R, W, start=True, stop=True, inc=None):
            return fw.op(fw.pe, lambda: nc.tensor.matmul(out, lhsT=lhsT, rhs=rhs, start=start, stop=stop),
                         R, W, inc=(stop if inc is None else inc))

        def dma(out, in_, R, W, q="sp", waw=True, owner=None):
            h = nc.sync if q == "sp" else nc.gpsimd
            e = fw.sp if q == "sp" else fw.pool
            return fw.dma(e, lambda: h.dma_start(out=out, in_=in_), R, W, owner=owner, waw=waw)

        def dmaT(out, in_, R, W, waw=True, owner=None):
            return fw.dma(fw.sp, lambda: nc.sync.dma_start_transpose(out=out, in_=in_), R, W, owner=owner, waw=waw)

        def rsqrt_col(stack_tiles, src, R, dst, scale, n=1):
            tmp, btmp = stack_tiles
            act("activation", R=R, W=[btmp], out=tmp[:, 0:n], in_=src, func=AF.Sqrt, bias=EPS, scale=scale)
            return tmp, btmp

        bcreg = nc.gpsimd.alloc_register("bcreg")
        nc.gpsimd.reg_mov(bcreg, NSLOT - 1)
        ps = top.enter_context(nc.psum_tensor("ps", [128, 8, 512], F32))
        pb = [Buf("pb%d" % i) for i in range(8)]
        psf = ps[:].rearrange("p b n -> p (b n)")

        bP = Buf("params")
        ident_f = T(top, [128, 128], F32); tri_f = T(top, [128, 128], F32)
        ident_b = T(top, [128, 128], BF16); tri_b = T(top, [128, 128], BF16)
        ones_b = T(top, [128, 128], BF16); ones_f = T(top, [128, 128], F32); onesq = T(top, [128, 128], F32)
        eoff = T(top, [128, NE], F32); iota = T(top, [128, NE], F32)
        cc = T(top, [128, KD, 2], F32); scc = T(top, [128, KD, 2], F32)
        badac = T(top, [128, 2, 48], F32); g1c = T(top, [128, 2, KD], F32)
        brbc = T(top, [128, 2, NE], F32)
        wdwc = T(top, [128, 2, 2, 31], F32); cvp = T(top, [128, 2, 2, 3], F32)
        bsc = T(top, [128, 2, 4], F32)
        wr = T(top, [128, 2, KD, NE], F32)
        zt = T(top, [128, 256], BF16)
        for t_, s_ in ((ident_f, ident_in), (tri_f, tri_in), (eoff, eoff_in), (iota, iota_in), (cc, cc_in),
                       (badac, badac_in), (g1c, g1c_in), (brbc, brbc_in),
                       (wdwc, wdwc_in), (cvp, cvp_in), (bsc, bsc_in)):
            dma(t_[:], s_, R=[bIN], W=[bP], waw=False)
        for l in range(2):
            dma(wr[:, l], wr_in[l].rearrange("(k p) n -> p k n", p=128), R=[bIN], W=[bP], waw=False)
        bC = Buf("consts")
        dve("tensor_copy", R=[bP], W=[bC], out=ident_b[:], in_=ident_f[:])
        dve("tensor_copy", R=[bP], W=[bC], out=tri_b[:], in_=tri_f[:])
        dve("memset", W=[bC], ap=ones_b[:], constant=1.0)
        dve("memset", W=[bC], ap=ones_f[:], constant=1.0)
        dve("memset", W=[bC], ap=onesq[:], constant=1.0 / 256.0)
        dve("memset", W=[bC], ap=zt[:], constant=0.0)
        act("activation", R=[bP], W=[bC], out=scc[:], in_=cc[:], func=AF.Silu)
        dma(US[0:16, :], zt[0:16, :], R=[bC], W=[bUS], waw=False)
        dma(US[16 + S:16 + S + 32, :], zt[0:32, :], R=[bC], W=[bUS], waw=False)
        dma(USC[0:16, :], zt[0:16, :], R=[bC], W=[bUSC], waw=False)
        dma(USC[16 + C:16 + C + 32, :], zt[0:32, :], R=[bC], W=[bUSC], waw=False)

        A1 = T(top, [128, 2, KD], F32); B1 = T(top, [128, 2, KD], F32)
        BC = T(top, [128, 2, 4, D], F32)
        bMOD = Buf("mod")
        sm = T(top, [128, 64], F32); bsm = Buf("sm")
        junk = T(top, [128, D], BF16); bjunk = Buf("junk")

        def compute_mod(l):
            with ExitStack() as ph:
                slab = [T(ph, [128, KD, 512], F32) for _ in range(2)]
                bslab = [Buf("slab%d" % i) for i in range(2)]
                brow = [T(ph, [1, 512], F32) for _ in range(2)]
                bbrow = [Buf("brow%d" % i) for i in range(2)]
                modc = T(ph, [128, 16, 2], F32); bmodc = Buf("modc")
                g2t = T(ph, [128, D], F32); bg2t = Buf("g2t")
                scb = T(ph, [128, KD, 2, 128], F32); bscb = Buf("scb")
                dve("tensor_copy", R=[bC], W=[bscb], out=scb[:], in_=scc[:].unsqueeze(3).to_broadcast([128, KD, 2, 128]))
                dma(g2t[:], g2bc_in[l], R=[bIN], W=[bg2t])
                nwhich = 2 if l == 0 else 1
                for sidx in range(12):
                    sl, bsl = slab[sidx % 2], bslab[sidx % 2]
                    dma(sl[:], wada_in[l, :, sidx * 512:(sidx + 1) * 512].rearrange("(k p) n -> p k n", p=128),
                        R=[bIN], W=[bsl])
                    if sidx < 4:
                        for jj in range(4):
                            j = sidx * 4 + jj
                            for k in range(KD):
                                mm(ps[:, 7, j * 2:j * 2 + 2], sl[:, k, jj * 128:(jj + 1) * 128], scc[:, k, :],
                                   R=[bsl, bC], W=[pb[7]], start=(k == 0), stop=(k == KD - 1))
                        if sidx == 3:
                            dve("tensor_tensor", R=[pb[7], bP], W=[bmodc], out=modc[:],
                                in0=ps[:, 7, 0:32].rearrange("p (j w) -> p j w", w=2),
                                in1=badac[:, l, 0:16].unsqueeze(2).to_broadcast([128, 16, 2]), op=ALU.add)
                            for w in range(2):
                                dve("scalar_tensor_tensor", R=[bmodc, bP], W=[bMOD], out=A1[:, w, :],
                                    in0=modc[:, 8:16, w], scalar=1.0, in1=g1c[:, l, :], op0=ALU.add, op1=ALU.mult)
                                dve("tensor_copy", R=[bmodc], W=[bMOD], out=B1[:, w, :], in_=modc[:, 0:8, w])
                    else:
                        v = (sidx - 4) // 2
                        half = (sidx - 4) % 2
                        br, bbr = brow[sidx % 2], bbrow[sidx % 2]
                        dma(br[:], bada_in[l:l + 1, sidx * 512:(sidx + 1) * 512], R=[bIN], W=[bbr])
                        for w in range(nwhich):
                            bank = 5 + w
                            for k in range(KD):
                                mm(ps[:, bank, :], scb[:, k, w, :], sl[:, k, :], R=[bsl, bscb], W=[pb[bank]],
                                   start=(k == 0), stop=False)
                            mm(ps[:, bank, :], ones_f[0:1, :], br[0:1, :], R=[bbr, bC], W=[pb[bank]],
                               start=False, stop=True)
                            dst = BC[:, w, v, half * 512:(half + 1) * 512]
                            if v == 2:
                                dve("scalar_tensor_tensor", R=[pb[bank], bg2t], W=[bMOD], out=dst,
                                    in0=ps[:, bank, :], scalar=1.0, in1=g2t[:, half * 512:(half + 1) * 512],
                                    op0=ALU.add, op1=ALU.mult)
                            else:
                                act("copy", R=[pb[bank]], W=[bMOD], out=dst, in_=ps[:, bank, :])
                fw.barrier()

        def phase1(l, ph, KT, VA, bKT, bVA):
            WIN = T(ph, [128, KD, INW], BF16); bWIN = Buf("WIN")
            gqk = T(ph, [128, 640], F32); cs = T(ph, [128, 32, 64], F32)
            sgp = T(ph, [128, 2, 256], F32); wsT = T(ph, [128, 4, 128], BF16)
            bP1 = Buf("p1params")
            dma(gqk[:], gqk_in[:, l, :], R=[bIN], W=[bP1], waw=False)
            dma(cs[:], cs_in, R=[bIN], W=[bP1], waw=False)
            dma(sgp[:], sgp_in[:, l], R=[bIN], W=[bP1], waw=False)
            dma(wsT[:], wsT_in[:, l], R=[bIN], W=[bP1], q="pool", waw=False)
            dma(WIN[:], win_in[l].rearrange("(k p) n -> p k n", p=128), R=[bIN], W=[bWIN], q="pool")
            xt = [T(ph, [128, D], F32) for _ in range(2)]; bxt = [Buf("xt%d" % i) for i in range(2)]
            xn = [T(ph, [128, D], BF16) for _ in range(2)]; bxn = [Buf("xn%d" % i) for i in range(2)]
            hT = [T(ph, [128, KD, 128], BF16) for _ in range(2)]; bhT = [Buf("hT%d" % i) for i in range(2)]
            sq = T(ph, [128, 640], F32); bsq = Buf("sq")
            qn = T(ph, [128, 640], F32); bqn = Buf("qn")
            rt = T(ph, [128, 4, 320], F32); brt = Buf("rt")
            qkb = [T(ph, [128, 640], BF16) for _ in range(2)]; bqkb = [Buf("qkb%d" % i) for i in range(2)]
            sig = T(ph, [128, 256], F32); bsig = Buf("sig")
            ub = [T(ph, [128, 256], BF16) for _ in range(2)]; bub = [Buf("ub%d" % i) for i in range(2)]
            zg = T(ph, [128, 512], F32); bzg = Buf("zg")
            vn = T(ph, [128, 256], F32); bvn = Buf("vn")
            vln = T(ph, [128, 256], BF16); bvln = Buf("vln")
            cob = [T(ph, [128, 256], BF16) for _ in range(2)]; bcob = [Buf("cob%d" % i) for i in range(2)]
            st6 = T(ph, [128, 8], F32); bst6 = Buf("st6")
            c1 = T(ph, [128, 32], F32); bc1 = Buf("c1")
            dve("memset", W=[bVA], ap=VA[:, :, :, 64:128], constant=1.0)

            def load(t):
                if l == 0:
                    src = x_in[t * 128:(t + 1) * 128, :] if t < 32 else ctx_in[(t - 32) * 128:(t - 31) * 128, :]
                    dma(xt[t % 2][:], src, R=[bIN], W=[bxt[t % 2]])
                else:
                    dma(xt[t % 2][:], XS[t * 128:(t + 1) * 128, :], R=[bXS], W=[bxt[t % 2]])
            load(0)
            for t in range(34):
                if t + 1 < 34:
                    load(t + 1)
                w = 0 if t < 32 else 1
                X, bX = xt[t % 2], bxt[t % 2]
                XN, bXN = xn[t % 2], bxn[t % 2]
                H, bH = hT[t % 2], bhT[t % 2]
                QB, bQB = qkb[t % 2], bqkb[t % 2]
                act("activation", R=[bX], W=[bjunk, bc1], out=junk[:], in_=X[:], func=AF.Square, accum_out=c1[:, 0:1])
                act("activation", R=[bc1], W=[bc1], out=c1[:, 1:2], in_=c1[:, 0:1], func=AF.Sqrt, bias=EPS, scale=1.0 / D)
                dve("reciprocal", R=[bc1], W=[bc1], out=c1[:, 2:3], in_=c1[:, 1:2])
                dve("tensor_scalar", R=[bX, bc1], W=[bXN], out=XN[:], in0=X[:], scalar1=c1[:, 2:3], scalar2=None, op0=ALU.mult)
                for k in range(KD):
                    mm(ps[:, k // 4, (k % 4) * 128:(k % 4 + 1) * 128], XN[:, k * 128:(k + 1) * 128], ident_b[:],
                       R=[bXN, bC], W=[pb[k // 4]], inc=(k % 4 == 3))
                for k in range(KD):
                    src = ps[:, k // 4, (k % 4) * 128:(k % 4 + 1) * 128]
                    if k % 2 == 0:
                        act("activation", R=[pb[k // 4], bMOD], W=[bH], out=H[:, k, :], in_=src, func=AF.Identity,
                            scale=A1[:, w, k:k + 1], bias=B1[:, w, k:k + 1])
                    else:
                        dve("tensor_scalar", R=[pb[k // 4], bMOD], W=[bH], out=H[:, k, :], in0=src,
                            scalar1=A1[:, w, k:k + 1], scalar2=B1[:, w, k:k + 1], op0=ALU.mult, op1=ALU.add)
                for n in range(4):
                    n0 = n * 512
                    wd_ = min(512, INW - n0)
                    for k in range(KD):
                        mm(ps[:, 2 + n, 0:wd_], H[:, k, :], WIN[:, k, n0:n0 + wd_], R=[bH, bWIN], W=[pb[2 + n]],
                           start=(k == 0), stop=(k == KD - 1))
                P0, P1, P2, P3 = ps[:, 2, :], ps[:, 3, :], ps[:, 4, :], ps[:, 5, :]
                act("activation", R=[pb[2]], W=[bsq], out=sq[:, 0:512], in_=P0, func=AF.Square)
                act("activation", R=[pb[3]], W=[bsq], out=sq[:, 512:640], in_=P1[:, 0:128], func=AF.Square)
                dve("tensor_reduce", R=[bsq], W=[bc1], out=c1[:, 4:14], in_=sq[:].rearrange("p (h d) -> p h d", d=64),
                    axis=AX.X, op=ALU.add)
                act("activation", R=[bc1], W=[bc1], out=c1[:, 14:24], in_=c1[:, 4:14], func=AF.Sqrt, bias=EPS, scale=1.0 / 64)
                dve("reciprocal", R=[bc1], W=[bc1], out=c1[:, 4:14], in_=c1[:, 14:24])
                dve("tensor_tensor", R=[pb[2], bc1], W=[bqn], out=qn[:, 0:512].rearrange("p (h d) -> p h d", d=64),
                    in0=P0.rearrange("p (h d) -> p h d", d=64),
                    in1=c1[:, 4:12].unsqueeze(2).to_broadcast([128, 8, 64]), op=ALU.mult)
                dve("tensor_tensor", R=[pb[3], bc1], W=[bqn], out=qn[:, 512:640].rearrange("p (h d) -> p h d", d=64),
                    in0=P1[:, 0:128].rearrange("p (h d) -> p h d", d=64),
                    in1=c1[:, 12:14].unsqueeze(2).to_broadcast([128, 2, 64]), op=ALU.mult)
                if t < 32:
                    pool("tensor_tensor", R=[bqn, bP1], W=[bqn], out=qn[:], in0=qn[:], in1=gqk[:], op=ALU.mult)
                    q3 = qn[:].rearrange("p (h d) -> p h d", d=64)
                    o3 = QB[:].rearrange("p (h d) -> p h d", d=64)
                    cosb = cs[:, t, 0:32].unsqueeze(1).to_broadcast([128, 10, 32])
                    sinb = cs[:, t, 32:64].unsqueeze(1).to_broadcast([128, 10, 32])
                    r3 = rt[:].rearrange("p a (h d) -> p a h d", d=32)
                    dve("tensor_tensor", R=[bqn, bP1], W=[brt], out=r3[:, 0], in0=q3[:, :, 0:32], in1=cosb, op=ALU.mult)
                    pool("tensor_tensor", R=[bqn, bP1], W=[brt], out=r3[:, 1], in0=q3[:, :, 32:64], in1=sinb, op=ALU.mult)
                    dve("tensor_tensor", R=[bqn, bP1], W=[brt], out=r3[:, 2], in0=q3[:, :, 32:64], in1=cosb, op=ALU.mult)
                    pool("tensor_tensor", R=[bqn, bP1], W=[brt], out=r3[:, 3], in0=q3[:, :, 0:32], in1=sinb, op=ALU.mult)
                    dve("tensor_tensor", R=[brt], W=[bQB], out=o3[:, :, 0:32], in0=r3[:, 0], in1=r3[:, 1], op=ALU.subtract)
                    pool("tensor_tensor", R=[brt], W=[bQB], out=o3[:, :, 32:64], in0=r3[:, 2], in1=r3[:, 3], op=ALU.add)
                else:
                    pool("tensor_tensor", R=[bqn, bP1], W=[bQB], out=QB[:], in0=qn[:], in1=gqk[:], op=ALU.mult)
                for j in range(4):
                    dma(QS[t * 128:(t + 1) * 128, j * 128:(j + 1) * 128].rearrange("p (g d) -> p g d", g=2),
                        QB[:, 0:512].rearrange("p (g j d) -> p j g d", g=2, j=4)[:, j], R=[bQB], W=[bQS], waw=False)
                dma(KS[t * 128:(t + 1) * 128, :], QB[:, 512:640], R=[bQB], W=[bKS], waw=False)
                act("copy", R=[pb[3]], W=[bVA], out=VA[:, t, :, 0:64], in_=P1[:, 128:256].rearrange("p (g d) -> p g d", d=64))
                UB, bUB = ub[t % 2], bub[t % 2]
                act("activation", R=[pb[4]], W=[bsig], out=sig[:], in_=P2[:, 0:256], func=AF.Sigmoid)
                dve("tensor_tensor", R=[pb[3], bsig], W=[bUB], out=UB[:], in0=P1[:, 256:512], in1=sig[:], op=ALU.mult)
                if t < 32:
                    dma(US[16 + t * 128:16 + (t + 1) * 128, :], UB[:], R=[bUB], W=[bUS], waw=False)
                else:
                    dma(USC[16 + (t - 32) * 128:16 + (t - 31) * 128, :], UB[:], R=[bUB], W=[bUSC], waw=False)
                act("activation", R=[pb[4], pb[5]], W=[bzg], out=zg[:], in_=psf[:, 4 * 512 + 256:5 * 512 + 256], func=AF.Gelu)
                dve("bn_stats", R=[bzg], W=[bst6], out=st6[:, 0:6], in_=zg[:, 256:512])
                dve("bn_aggr", R=[bst6], W=[bst6], out=st6[:, 6:8], in_=st6[:, 0:6])
                act("activation", R=[bst6], W=[bc1], out=c1[:, 24:25], in_=st6[:, 7:8], func=AF.Sqrt, bias=EPS, scale=1.0)
                dve("reciprocal", R=[bc1], W=[bc1], out=c1[:, 25:26], in_=c1[:, 24:25])
                dve("tensor_scalar", R=[bzg, bst6, bc1], W=[bvn], out=vn[:], in0=zg[:, 256:512], scalar1=st6[:, 6:7],
                    scalar2=c1[:, 25:26], op0=ALU.subtract, op1=ALU.mult)
                pool("tensor_tensor", R=[bvn, bP1], W=[bvn], out=vn[:], in0=vn[:], in1=sgp[:, 0, :], op=ALU.mult)
                pool("tensor_tensor", R=[bvn, bP1], W=[bvln], out=vln[:], in0=vn[:], in1=sgp[:, 1, :], op=ALU.add)
                for h in range(4):
                    mm(ps[:, 6, h * 64:(h + 1) * 64], wsT[:, h, :], vln[:, h * 64:(h + 1) * 64], R=[bvln, bP1], W=[pb[6]],
                       inc=(h == 3))
                CO, bCO = cob[t % 2], bcob[t % 2]
                for h in range(4):
                    dve("scalar_tensor_tensor", R=[pb[6], bzg, bP], W=[bCO], out=CO[:, h * 64:(h + 1) * 64],
                        in0=ps[:, 6, h * 64:(h + 1) * 64], scalar=bsc[:, l, h:h + 1], in1=zg[:, h * 64:(h + 1) * 64],
                        op0=ALU.add, op1=ALU.mult)
                dma(CS[t * 128:(t + 1) * 128, :], CO[:], R=[bCO], W=[bCS], waw=False)
            for i in range(34):
                dmaT(KT[:, i * 128:(i + 1) * 128], KS[i * 128:(i + 1) * 128, :], R=[bKS], W=[bKT], waw=False)

        def phase2(l, ph, KT, VA, bKT, bVA, DEST, W4, bRT):
            WO = T(ph, [128, KD, D], BF16); bWO = Buf("WO")
            dma(WO[:], wo_in[l].rearrange("(k p) n -> p k n", p=128), R=[bIN], W=[bWO], q="pool")
            DG = T(ph, [128, 2, 31, 128], BF16); bDG = Buf("DG")
            for c_ in range(2):
                for k in range(31):
                    e_ = dve if (k % 2 == 0) else pool
                    e_("tensor_scalar", R=[bC, bP], W=[bDG], out=DG[:, c_, k, :], in0=ident_f[:],
                       scalar1=wdwc[:, l, c_, k:k + 1], scalar2=None, op0=ALU.mult)
            QTb = [T(ph, [128, 4, 512], BF16) for _ in range(2)]; bQTb = [Buf("QTb%d" % i) for i in range(2)]
            UTb = [T(ph, [128, 2, 544], BF16) for _ in range(2)]; bUTb = [Buf("UTb%d" % i) for i in range(2)]
            CTb = [T(ph, [128, 2, 512], BF16) for _ in range(2)]; bCTb = [Buf("CTb%d" % i) for i in range(2)]
            cv = T(ph, [128, 2, 512], F32); bcv = Buf("cv")
            sqv = T(ph, [128, 2, 512], F32); bsqv = Buf("sqv")
            msq = T(ph, [128, 512], F32); bmsq = Buf("msq")
            rstd = T(ph, [128, 512], F32); brstd = Buf("rstd")
            CVb = T(ph, [128, 2, 512], BF16); bCVb = Buf("CVb")
            PT = [T(ph, [128, 512], BF16) for _ in range(3)]; bPT = [Buf("PT%d" % i) for i in range(3)]
            AT = T(ph, [128, 4, 512], BF16); bAT = Buf("AT")
            rec = T(ph, [64, 512], F32); brec = Buf("rec")
            xr = [T(ph, [128, D], F32) for _ in range(2)]; bxr = [Buf("xr%d" % i) for i in range(2)]
            xm = [T(ph, [128, D], F32) for _ in range(2)]; bxm = [Buf("xm%d" % i) for i in range(2)]
            tmp = T(ph, [128, D], F32); btmp = Buf("tmp")
            h2 = T(ph, [128, D], F32); bh2 = Buf("h2")
            h2b = [T(ph, [128, D], BF16) for _ in range(2)]; bh2b = [Buf("h2b%d" % i) for i in range(2)]
            h2T = T(ph, [128, KD, 128], F32); bh2T = Buf("h2T")
            c1 = T(ph, [128, 8], F32); bc1 = Buf("c1b")
            lg = T(ph, [128, NE], F32); blg = Buf("lg")
            mx8 = T(ph, [128, 8], F32); bmx8 = Buf("mx8")
            ix8 = T(ph, [128, 8], U32); bix8 = Buf("ix8")
            ixf = T(ph, [128, 8], F32); bixf = Buf("ixf")
            e4 = T(ph, [128, 8], F32); be4 = Buf("e4")
            mask = T(ph, [128, NE], BF16); bmask = Buf("mask")
            cnt = T(ph, [128, NE], F32); bcnt = Buf("cnt")
            posf = T(ph, [128, NE], F32); bposf = Buf("posf")
            val = T(ph, [128, NE], F32); bval = Buf("val")
            oh = T(ph, [128, 4, NE], F32); boh = Buf("oh")
            dk = T(ph, [128, 8], F32); bdk = Buf("dk")
            dve("memset", W=[bcnt], ap=cnt[:], constant=0.0)

            blocks = [(b * 512, 512, list(range(34)), 0) for b in range(8)]
            if l == 0:
                blocks.append((S, 256, [32, 33], 1))

            def loads(bi):
                tok0, nt, kcs, w = blocks[bi]
                Q, bQ = QTb[bi % 2], bQTb[bi % 2]
                U, bU = UTb[bi % 2], bUTb[bi % 2]
                Cc, bCc = CTb[bi % 2], bCTb[bi % 2]
                for j in range(4):
                    dmaT(Q[:, j, 0:nt], QS[tok0:tok0 + nt, j * 128:(j + 1) * 128], R=[bQS], W=[bQ], waw=(j == 0))
                for c_ in range(2):
                    if w == 0:
                        dmaT(U[:, c_, 0:nt + 32], US[tok0:tok0 + nt + 32, c_ * 128:(c_ + 1) * 128], R=[bUS], W=[bU], waw=(c_ == 0))
                    else:
                        dmaT(U[:, c_, 0:nt + 32], USC[0:nt + 32, c_ * 128:(c_ + 1) * 128], R=[bUSC], W=[bU], waw=(c_ == 0))
                    dmaT(Cc[:, c_, 0:nt], CS[tok0:tok0 + nt, c_ * 128:(c_ + 1) * 128], R=[bCS], W=[bCc], waw=(c_ == 0))

            loads(0)
            for bi, (tok0, nt, kcs, w) in enumerate(blocks):
                if bi + 1 < len(blocks):
                    loads(bi + 1)
                Q, bQ = QTb[bi % 2], bQTb[bi % 2]
                U, bU = UTb[bi % 2], bUTb[bi % 2]
                Cc, bCc = CTb[bi % 2], bCTb[bi % 2]
                for c_ in range(2):
                    for k in range(31):
                        mm(ps[:, c_, 0:nt], DG[:, c_, k, :], U[:, c_, 1 + k:1 + k + nt], R=[bDG, bU], W=[pb[c_]],
                           start=(k == 0), stop=(k == 30))
                    act("activation", R=[pb[c_], bP], W=[bcv], out=cv[:, c_, 0:nt], in_=ps[:, c_, 0:nt], func=AF.Identity,
                        bias=cvp[:, l, c_, 0:1], scale=1.0)
                    act("activation", R=[bcv], W=[bsqv], out=sqv[:, c_, 0:nt], in_=cv[:, c_, 0:nt], func=AF.Square)
                for c_ in range(2):
                    mm(ps[:, 2, 0:nt], onesq[:], cv[:, c_, 0:nt], R=[bcv, bC], W=[pb[2]], start=(c_ == 0), stop=(c_ == 1))
                for c_ in range(2):
                    mm(ps[:, 0, 0:nt], onesq[:], sqv[:, c_, 0:nt], R=[bsqv, bC], W=[pb[0]], start=(c_ == 0), stop=(c_ == 1))
                act("activation", R=[pb[2]], W=[bmsq], out=msq[:, 0:nt], in_=ps[:, 2, 0:nt], func=AF.Square)
                dve("tensor_tensor", R=[pb[0], bmsq], W=[bmsq], out=msq[:, 0:nt], in0=ps[:, 0, 0:nt], in1=msq[:, 0:nt], op=ALU.subtract)
                act("activation", R=[bmsq], W=[brstd], out=rstd[:, 0:nt], in_=msq[:, 0:nt], func=AF.Sqrt, bias=EPS, scale=1.0)
                dve("reciprocal", R=[brstd], W=[brstd], out=rstd[:, 0:nt], in_=rstd[:, 0:nt])
                for c_ in range(2):
                    dve("tensor_tensor", R=[bcv, pb[2]], W=[bcv], out=cv[:, c_, 0:nt], in0=cv[:, c_, 0:nt], in1=ps[:, 2, 0:nt], op=ALU.subtract)
                    pool("tensor_tensor", R=[bcv, brstd], W=[bcv], out=cv[:, c_, 0:nt], in0=cv[:, c_, 0:nt], in1=rstd[:, 0:nt], op=ALU.mult)
                    act("activation", R=[bcv, bP], W=[bCVb], out=CVb[:, c_, 0:nt], in_=cv[:, c_, 0:nt], func=AF.Silu,
                        scale=cvp[:, l, c_, 1:2], bias=cvp[:, l, c_, 2:3])
                steps = [(h, i) for h in range(8) for i in range(len(kcs))]

                def score(si):
                    h, i = steps[si]
                    g, j = h // 4, h % 4
                    kc = kcs[i]
                    r = si % 3
                    mm(ps[:, r, 0:nt], KT[g * 64:(g + 1) * 64, kc * 128:(kc + 1) * 128], Q[g * 64:(g + 1) * 64, j, 0:nt],
                       R=[bKT, bQ], W=[pb[r]])
                score(0)
                if len(steps) > 1:
                    score(1)
                for si, (h, i) in enumerate(steps):
                    g, j = h // 4, h % 4
                    kc = kcs[i]
                    r = si % 3
                    ob = 3 + (h % 2)
                    act("activation", R=[pb[r]], W=[bPT[r]], out=PT[r][:, 0:nt], in_=ps[:, r, 0:nt], func=AF.Exp, scale=0.125)
                    mm(ps[:, ob, 0:nt], VA[:, kc, g, :], PT[r][:, 0:nt], R=[bVA, bPT[r]], W=[pb[ob]],
                       start=(i == 0), stop=(i == len(kcs) - 1), inc=True)
                    if si + 2 < len(steps):
                        score(si + 2)
                    if i == len(kcs) - 1:
                        dve("reciprocal", R=[pb[ob]], W=[brec], out=rec[:, 0:nt], in_=ps[64:128, ob, 0:nt])
                        dve("tensor_tensor", R=[pb[ob], brec], W=[bAT], out=AT[(h % 2) * 64:(h % 2) * 64 + 64, h // 2, 0:nt],
                            in0=ps[0:64, ob, 0:nt], in1=rec[:, 0:nt], op=ALU.mult)
                for s_ in range(nt // 128):
                    t = (tok0 // 128) + s_
                    cs_ = slice(s_ * 128, (s_ + 1) * 128)
                    XR, bXR = xr[t % 2], bxr[t % 2]
                    XMt, bXMt = xm[t % 2], bxm[t % 2]
                    H2B, bH2B = h2b[t % 2], bh2b[t % 2]
                    if l == 0:
                        src = x_in[t * 128:(t + 1) * 128, :] if t < 32 else ctx_in[(t - 32) * 128:(t - 31) * 128, :]
                        dma(XR[:], src, R=[bIN], W=[bXR])
                    else:
                        dma(XR[:], XS[t * 128:(t + 1) * 128, :], R=[bXS], W=[bXR])
                    for n in range(2):
                        for kk in range(KD):
                            if kk < 4:
                                lh, bl = AT[:, kk, cs_], bAT
                            elif kk < 6:
                                lh, bl = CVb[:, kk - 4, cs_], bCVb
                            else:
                                lh, bl = Cc[:, kk - 6, cs_], bCc
                            mm(ps[:, 5 + n, :], lh, WO[:, kk, n * 512:(n + 1) * 512], R=[bl, bWO], W=[pb[5 + n]],
                               start=(kk == 0), stop=(kk == KD - 1))
                    dve("tensor_tensor", R=[pb[5], pb[6], bMOD], W=[btmp], out=tmp[:], in0=psf[:, 5 * 512:7 * 512], in1=BC[:, w, 0, :], op=ALU.mult)
                    pool("tensor_tensor", R=[btmp, bXR], W=[bXMt], out=XMt[:], in0=tmp[:], in1=XR[:], op=ALU.add)
                    dma(XM[t * 128:(t + 1) * 128, :], XMt[:], R=[bXMt], W=[bXM], waw=False)
                    act("activation", R=[bXMt], W=[bjunk, bc1], out=junk[:], in_=XMt[:], func=AF.Square, accum_out=c1[:, 0:1])
                    act("activation", R=[bc1], W=[bc1], out=c1[:, 1:2], in_=c1[:, 0:1], func=AF.Sqrt, bias=EPS, scale=1.0 / D)
                    dve("reciprocal", R=[bc1], W=[bc1], out=c1[:, 2:3], in_=c1[:, 1:2])
                    dve("scalar_tensor_tensor", R=[bXMt, bc1, bMOD], W=[btmp], out=tmp[:], in0=XMt[:], scalar=c1[:, 2:3], in1=BC[:, w, 2, :],
                        op0=ALU.mult, op1=ALU.mult)
                    pool("tensor_tensor", R=[btmp, bMOD], W=[bh2], out=h2[:], in0=tmp[:], in1=BC[:, w, 1, :], op=ALU.add)
                    act("copy", R=[bh2], W=[bH2B], out=H2B[:], in_=h2[:])
                    for k in range(KD):
                        mm(ps[:, k // 4, (k % 4) * 128:(k % 4 + 1) * 128], h2[:, k * 128:(k + 1) * 128], ident_f[:],
                           R=[bh2, bP], W=[pb[k // 4]], inc=(k % 4 == 3))
                    act("copy", R=[pb[0]], W=[bh2T], out=h2T[:, 0:4, :], in_=ps[:, 0, :].rearrange("p (k n) -> p k n", n=128))
                    dve("tensor_copy", R=[pb[1]], W=[bh2T], out=h2T[:, 4:8, :], in_=ps[:, 1, :].rearrange("p (k n) -> p k n", n=128))
                    for k in range(KD):
                        mm(ps[:, 7, 0:NE], h2T[:, k, :], wr[:, l, k, :], R=[bh2T, bP], W=[pb[7]], start=(k == 0), stop=(k == KD - 1))
                    dve("tensor_tensor", R=[pb[7], bP], W=[blg], out=lg[:], in0=ps[:, 7, 0:NE], in1=brbc[:, l, :], op=ALU.add)
                    dve("max", R=[blg], W=[bmx8], out=mx8[:], in_=lg[:])
                    dve("max_index", R=[blg, bmx8], W=[bix8], out=ix8[:], in_max=mx8[:], in_values=lg[:])
                    dve("tensor_copy", R=[bix8], W=[bixf], out=ixf[:], in_=ix8[:])
                    dve("tensor_scalar", R=[bmx8], W=[be4], out=e4[:, 4:5], in0=mx8[:, 0:1], scalar1=-1.0, scalar2=None, op0=ALU.mult)
                    act("activation", R=[bmx8, be4], W=[be4], out=e4[:, 0:4], in_=mx8[:, 0:4], func=AF.Exp, bias=e4[:, 4:5], scale=1.0,
                        accum_out=e4[:, 5:6])
                    dve("reciprocal", R=[be4], W=[be4], out=e4[:, 6:7], in_=e4[:, 5:6])
                    dve("tensor_scalar", R=[blg, bmx8], W=[bmask], out=mask[:], in0=lg[:], scalar1=mx8[:, 3:4], scalar2=None, op0=ALU.is_ge)
                    mm(ps[:, 7, 32:64], tri_b[:], mask[:], R=[bmask, bC], W=[pb[7]])
                    mm(ps[:, 7, 64:96], ones_b[:], mask[:], R=[bmask, bC], W=[pb[7]])
                    dve("tensor_tensor", R=[pb[7], bcnt], W=[bposf], out=posf[:], in0=ps[:, 7, 32:64], in1=cnt[:], op=ALU.add)
                    dve("tensor_tensor", R=[pb[7], bcnt], W=[bcnt], out=cnt[:], in0=ps[:, 7, 64:96], in1=cnt[:], op=ALU.add)
                    dve("tensor_scalar", R=[bposf], W=[bval], out=val[:], in0=posf[:], scalar1=float(CAP), scalar2=None, op0=ALU.is_lt)
                    dve("tensor_tensor", R=[bposf, bP], W=[bposf], out=posf[:], in0=posf[:], in1=eoff[:], op=ALU.add)
                    dve("tensor_scalar", R=[bposf], W=[bposf], out=posf[:], in0=posf[:], scalar1=-BIG, scalar2=None, op0=ALU.add)
                    dve("tensor_tensor", R=[bposf, bval], W=[bposf], out=posf[:], in0=posf[:], in1=val[:], op=ALU.mult)
                    dve("tensor_scalar", R=[bposf], W=[bposf], out=posf[:], in0=posf[:], scalar1=BIG, scalar2=None, op0=ALU.add)
                    for k in range(4):
                        dve("tensor_scalar", R=[bixf, bP], W=[boh], out=oh[:, k, :], in0=iota[:], scalar1=ixf[:, k:k + 1], scalar2=None,
                            op0=ALU.is_equal)
                    dve("tensor_tensor", R=[boh, bposf], W=[boh], out=oh[:], in0=oh[:], in1=posf[:].unsqueeze(1).to_broadcast([128, 4, NE]),
                        op=ALU.mult)
                    dve("tensor_reduce", R=[boh], W=[bdk], out=dk[:, 0:4], in_=oh[:], axis=AX.X, op=ALU.add)
                    dve("tensor_copy", R=[bdk], W=[bRT], out=DEST[:, t, :], in_=dk[:, 0:4])
                    dve("tensor_scalar", R=[bdk], W=[bdk], out=dk[:, 4:8], in0=dk[:, 0:4], scalar1=float(NSLOT), scalar2=None, op0=ALU.is_lt)
                    dve("tensor_scalar", R=[be4], W=[be4], out=e4[:, 0:4], in0=e4[:, 0:4], scalar1=e4[:, 6:7], scalar2=None, op0=ALU.mult)
                    dve("tensor_tensor", R=[be4, bdk], W=[bRT], out=W4[:, t, :], in0=e4[:, 0:4], in1=dk[:, 4:8], op=ALU.mult)
                    for k in range(4):
                        fw.dma(fw.pool, lambda k=k: nc.gpsimd.indirect_dma_start(
                            out=XG, out_offset=bass.IndirectOffsetOnAxis(ap=DEST[:, t, k:k + 1], axis=0),
                            in_=H2B[:], in_offset=None, bounds_check=bcreg, oob_is_err=False),
                            reads=[bH2B, bRT], writes=[bXG], waw=False)

        def phase3(l, ph):
            NB = CAP // 512
            WB = [T(ph, [128, KD, D], BF16) for _ in range(4)]; bWB = [Buf("WB%d" % i) for i in range(4)]
            BD = [T(ph, [1, D], BF16) for _ in range(2)]; bBD = [Buf("BD%d" % i) for i in range(2)]
            bgu = T(ph, [128, NE, 2, KD], F32); bBGU = Buf("bgu")
            dma(bgu[:], bgu_in[:, l], R=[bIN], W=[bBGU])
            xT = [T(ph, [128, KD, 512], BF16) for _ in range(2)]; bxT = [Buf("xT%d" % i) for i in range(2)]
            aT = [T(ph, [128, KD, 512], BF16) for _ in range(2)]; baT = [Buf("aT%d" % i) for i in range(2)]
            gb = [T(ph, [128, 512], F32) for _ in range(2)]; bgb = [Buf("gb%d" % i) for i in range(2)]
            sg = [T(ph, [128, 512], F32) for _ in range(2)]; bsg = [Buf("sg%d" % i) for i in range(2)]
            ubt = [T(ph, [128, 512], F32) for _ in range(2)]; bubt = [Buf("ubt%d" % i) for i in range(2)]
            yt = [T(ph, [128, D], F32) for _ in range(2)]; byt = [Buf("yt%d" % i) for i in range(2)]
            wsrc = (wg_in, wu_in, wd_in)

            def mload(m):
                e, j = m // 3, m % 3
                dma(WB[m % 4][:], wsrc[j][l, e].rearrange("(k p) n -> p k n", p=128), R=[bIN], W=[bWB[m % 4]], q="pool")
                if j == 2:
                    dma(BD[e % 2][:], bd_in[l, e:e + 1, :], R=[bIN], W=[bBD[e % 2]], q="pool")

            items = [(e, b) for e in range(NE) for b in range(NB)]

            def xload(ii):
                e, b = items[ii]
                r0 = e * CAP + b * 512
                for k in range(KD):
                    dmaT(xT[ii % 2][:, k, :], XG[r0:r0 + 512, k * 128:(k + 1) * 128], R=[bXG], W=[bxT[ii % 2]], waw=(k == 0))
            nload = [0]

            def prefetch(upto):
                while nload[0] <= upto and nload[0] < 3 * NE:
                    mload(nload[0])
                    nload[0] += 1
            prefetch(2)
            xload(0)
            fcount = 0
            ycount = 0
            for ii, (e, b) in enumerate(items):
                WGe, bWGe = WB[(3 * e) % 4], bWB[(3 * e) % 4]
                WUe, bWUe = WB[(3 * e + 1) % 4], bWB[(3 * e + 1) % 4]
                WDe, bWDe = WB[(3 * e + 2) % 4], bWB[(3 * e + 2) % 4]
                if b == 0:
                    prefetch(3 * e + 3)
                if ii + 1 < len(items):
                    xload(ii + 1)
                X, bX = xT[ii % 2], bxT[ii % 2]
                A, bA = aT[ii % 2], baT[ii % 2]
                for f in range(KD):
                    pg = (fcount % 2) * 2
                    fcount += 1
                    for k in range(KD):
                        mm(ps[:, pg, :], WGe[:, k, f * 128:(f + 1) * 128], X[:, k, :], R=[bWGe, bX], W=[pb[pg]],
                           start=(k == 0), stop=(k == KD - 1))
                    for k in range(KD):
                        mm(ps[:, pg + 1, :], WUe[:, k, f * 128:(f + 1) * 128], X[:, k, :], R=[bWUe, bX], W=[pb[pg + 1]],
                           start=(k == 0), stop=(k == KD - 1))
                    G, bG = gb[f % 2], bgb[f % 2]
                    SG, bSG = sg[f % 2], bsg[f % 2]
                    UU, bUU = ubt[f % 2], bubt[f % 2]
                    dve("tensor_scalar", R=[pb[pg], bBGU], W=[bG], out=G[:], in0=ps[:, pg, :], scalar1=bgu[:, e, 0, f:f + 1], scalar2=7.0,
                        op0=ALU.add, op1=ALU.min)
                    act("activation", R=[bG], W=[bSG], out=SG[:], in_=G[:], func=AF.Sigmoid, scale=1.702)
                    dve("tensor_scalar", R=[pb[pg + 1], bBGU], W=[bUU], out=UU[:], in0=ps[:, pg + 1, :], scalar1=bgu[:, e, 1, f:f + 1],
                        scalar2=7.0, op0=ALU.add, op1=ALU.min)
                    pool("tensor_scalar", R=[bUU], W=[bUU], out=UU[:], in0=UU[:], scalar1=-7.0, scalar2=1.0, op0=ALU.max, op1=ALU.add)
                    pool("tensor_tensor", R=[bG, bSG], W=[bSG], out=SG[:], in0=G[:], in1=SG[:], op=ALU.mult)
                    dve("tensor_tensor", R=[bUU, bSG], W=[bA], out=A[:, f, :], in0=UU[:], in1=SG[:], op=ALU.mult)
                for s4 in range(4):
                    Y, bY = yt[ycount % 2], byt[ycount % 2]
                    pbase = 4 + (ycount % 2) * 2
                    ycount += 1
                    for n in range(2):
                        for f in range(KD):
                            mm(ps[:, pbase + n, :], A[:, f, s4 * 128:(s4 + 1) * 128], WDe[:, f, n * 512:(n + 1) * 512],
                               R=[bA, bWDe], W=[pb[pbase + n]], start=(f == 0), stop=False)
                        mm(ps[:, pbase + n, :], ones_b[0:1, :], BD[e % 2][0:1, n * 512:(n + 1) * 512], R=[bBD[e % 2], bC], W=[pb[pbase + n]],
                           start=False, stop=True)
                    act("copy", R=[pb[pbase]], W=[bY], out=Y[:, 0:512], in_=ps[:, pbase, :])
                    dve("tensor_copy", R=[pb[pbase + 1]], W=[bY], out=Y[:, 512:1024], in_=ps[:, pbase + 1, :])
                    r0 = e * CAP + b * 512 + s4 * 128
                    dma(YG[r0:r0 + 128, :], Y[:], R=[bY], W=[bYG], waw=False)

        def phase4(l, ph, DEST, W4, bRT):
            last = (l == nlayers - 1)
            Y4 = [T(ph, [128, 4, D], F32) for _ in range(2)]; bY4 = [Buf("Y4%d" % i) for i in range(2)]
            xmt = [T(ph, [128, D], F32) for _ in range(2)]; bxmt = [Buf("xmt%d" % i) for i in range(2)]
            acc = T(ph, [128, D], F32); bacc = Buf("acc")
            xo = [T(ph, [128, D], F32) for _ in range(2)]; bxo = [Buf("xo%d" % i) for i in range(2)]
            c1 = T(ph, [128, 8], F32); bc1 = Buf("c1c")
            gfbc = T(ph, [128, D], F32); bGF = Buf("gfbc")
            if last:
                dma(gfbc[:], gfbc_in, R=[bIN], W=[bGF])
            for i in range(2):
                pool("memset", W=[bY4[i]], ap=Y4[i][:], constant=0.0)
            ntile = 32 if last else 34

            def loads(t):
                dma(xmt[t % 2][:], XM[t * 128:(t + 1) * 128, :], R=[bXM], W=[bxmt[t % 2]])
                for k in range(4):
                    fw.dma(fw.pool, lambda k=k: nc.gpsimd.indirect_dma_start(
                        out=Y4[t % 2][:, k, :], out_offset=None, in_=YG,
                        in_offset=bass.IndirectOffsetOnAxis(ap=DEST[:, t, k:k + 1], axis=0),
                        bounds_check=bcreg, oob_is_err=False),
                        reads=[bYG, bRT], writes=[bY4[t % 2]], waw=(k == 0))
            loads(0)
            for t in range(ntile):
                if t + 1 < ntile:
                    loads(t + 1)
                w = 0 if t < 32 else 1
                Y, bY = Y4[t % 2], bY4[t % 2]
                XMt, bXMt = xmt[t % 2], bxmt[t % 2]
                XO, bXO = xo[t % 2], bxo[t % 2]
                dve("tensor_scalar", R=[bY, bRT], W=[bacc], out=acc[:], in0=Y[:, 0, :], scalar1=W4[:, t, 0:1], scalar2=None, op0=ALU.mult)
                for k in range(1, 4):
                    dve("scalar_tensor_tensor", R=[bY, bRT, bacc], W=[bacc], out=acc[:], in0=Y[:, k, :], scalar=W4[:, t, k:k + 1], in1=acc[:],
                       op0=ALU.mult, op1=ALU.add)
                pool("tensor_tensor", R=[bacc, bMOD], W=[bacc], out=acc[:], in0=acc[:], in1=BC[:, w, 3, :], op=ALU.mult)
                dve("tensor_tensor", R=[bacc, bXMt], W=[bXO], out=XO[:], in0=acc[:], in1=XMt[:], op=ALU.add)
                if not last:
                    dma(XS[t * 128:(t + 1) * 128, :], XO[:], R=[bXO], W=[bXS], waw=False)
                else:
                    act("activation", R=[bXO], W=[bjunk, bc1], out=junk[:], in_=XO[:], func=AF.Square, accum_out=c1[:, 0:1])
                    act("activation", R=[bc1], W=[bc1], out=c1[:, 1:2], in_=c1[:, 0:1], func=AF.Sqrt, bias=EPS, scale=1.0 / D)
                    dve("reciprocal", R=[bc1], W=[bc1], out=c1[:, 2:3], in_=c1[:, 1:2])
                    dve("scalar_tensor_tensor", R=[bXO, bc1, bGF], W=[bXO], out=XO[:], in0=XO[:], scalar=c1[:, 2:3], in1=gfbc[:],
                        op0=ALU.mult, op1=ALU.mult)
                    dma(out_d[t * 128:(t + 1) * 128, :], XO[:], R=[bXO], W=[bOUT], waw=False)

        done = False
        for l in range(nlayers):
            compute_mod(l)
            with ExitStack() as lay:
                DEST = T(lay, [128, 34, 4], I32); W4 = T(lay, [128, 34, 4], F32); bRT = Buf("route")
                with ExitStack() as att:
                    KT = T(att, [128, NTOK], BF16); bKT = Buf("KT")
                    VA = T(att, [128, 34, 2, 128], BF16); bVA = Buf("VA")
                    with ExitStack() as ph:
                        phase1(l, ph, KT, VA, bKT, bVA)
                        fw.barrier()
                    if stop_after == (l, 1):
                        done = True
                    if not done:
                        with ExitStack() as ph:
                            phase2(l, ph, KT, VA, bKT, bVA, DEST, W4, bRT)
                            fw.barrier()
                        if stop_after == (l, 2):
                            done = True
                if not done:
                    with ExitStack() as ph:
                        phase3(l, ph)
                        fw.barrier()
                    if stop_after == (l, 3):
                        done = True
                if not done:
                    with ExitStack() as ph:
                        phase4(l, ph, DEST, W4, bRT)
                        fw.barrier()
            if done:
                break
        fw.barrier()
    return nc


def _rope_tables():
    pos = np.arange(S)
    r = (pos // 64).astype(np.float32)
    col = (pos % 64).astype(np.float32)
    inv = (10000.0 ** (-np.arange(0, 32, 2, dtype=np.float32) / 32.0)).astype(np.float32)
    ang = np.concatenate([r[:, None] * inv[None, :], col[:, None] * inv[None, :]], axis=-1).astype(np.float32)
    tab = np.concatenate([np.cos(ang), np.sin(ang)], axis=-1).astype(np.float32)
    return np.ascontiguousarray(tab.reshape(32, 128, 64).transpose(1, 0, 2))


def _col(v):
    v = np.asarray(v, dtype=np.float32)
    lead = v.shape[:-1]
    n = v.shape[-1] // 128
    a = v.reshape(*lead, n, 128)
    return np.ascontiguousarray(np.moveaxis(a, -1, 0))


def make_in_maps(inp, cores):
    f = lambda a: np.ascontiguousarray(np.asarray(a, dtype=np.float32))
    bc = lambda v: np.ascontiguousarray(np.broadcast_to(np.asarray(v, np.float32), (128,) + np.asarray(v).shape))
    shared = {
        "w_ada": f(inp["w_ada"]), "b_ada": f(inp["b_ada"]),
        "b_adac": _col(inp["b_ada"]),
        "g1c": _col(inp["g_norm1"]),
        "g2bc": np.ascontiguousarray(np.broadcast_to(f(inp["g_norm2"])[:, None, :], (2, 128, D))),
        "gfbc": bc(inp["g_final"]),
        "w_in": f(inp["w_in"]), "w_o": f(inp["w_o"]), "w_router": f(inp["w_router"]),
        "b_rbc": bc(inp["b_router"]),
        "gqk": bc(np.concatenate([np.tile(f(inp["g_q"]), (1, 8)), np.tile(f(inp["g_k"]), (1, 2))], axis=1)),
        "cs": _rope_tables(),
        "wdwc": np.ascontiguousarray(_col(inp["w_dw"])),
        "cvp": np.ascontiguousarray(np.stack([_col(inp["b_dw"]), _col(inp["g_conv_ln"]), _col(inp["b_conv_ln"])], axis=-1)),
        "sgp": bc(np.stack([f(inp["g_sgu_ln"]), f(inp["b_sgu_ln"])], axis=1)),
        "wsT": np.ascontiguousarray(f(inp["w_s"]).transpose(3, 0, 1, 2)),
        "bsc": np.ascontiguousarray(f(inp["b_s"]).transpose(2, 0, 1)),
        "w_gate": f(inp["w_gate"]), "w_up": f(inp["w_up"]), "w_down": f(inp["w_down"]),
        "bgu": np.ascontiguousarray(np.stack([_col(inp["b_gate"]), _col(inp["b_up"])], axis=3)),
        "b_down": f(inp["b_down"]),
        "ident": np.eye(128, dtype=np.float32),
        "tri": np.triu(np.ones((128, 128), np.float32), k=1),
        "eoff": bc(np.arange(NE, dtype=np.float32) * CAP),
        "iota": bc(np.arange(NE, dtype=np.float32)),
    }
    shared["wdwc"] = np.ascontiguousarray(shared["wdwc"].transpose(0, 1, 3, 2))
    maps = []
    for b in cores:
        m = dict(shared)
        m["x"] = f(inp["x"][b]); m["ctx"] = f(inp["ctx"][b])
        m["cc"] = np.ascontiguousarray(np.stack([_col(inp["c"][b]), _col(inp["c_ctx"])], axis=-1))
        maps.append(m)
    return maps


_NC = None


def kernel(**inputs):
    global _NC
    if _NC is None:
        _NC = build()
    maps = make_in_maps(inputs, list(range(8)))
    res = run_bass_kernel_spmd(_NC, maps, core_ids=list(range(8)))
    return np.stack([np.asarray(r["out"], dtype=np.float32) for r in res.results], axis=0)
```

```python
import math
import numpy as np
from contextlib import ExitStack
import concourse.bass as bass
import concourse.mybir as mybir
from concourse.bass_utils import run_bass_kernel_spmd

F32 = mybir.dt.float32
BF16 = mybir.dt.bfloat16
I32 = mybir.dt.int32
U32 = mybir.dt.uint32
AF = mybir.ActivationFunctionType
ALU = mybir.AluOpType
AX = mybir.AxisListType

S = 4096
C = 256
NTOK = S + C
D = 1024
KD = 8
INW = 1792
NE = 32
CAP = 2048
NSLOT = NE * CAP
EPS = 1e-6
BIG = 4.0e6
CSIG = float(np.float32(1.0 / (1.0 + math.exp(-1.702 * 7.0))))


class Buf:
    __slots__ = ("name", "w", "r")

    def __init__(self, name=""):
        self.name = name
        self.w = {}
        self.r = {}


class Eng:
    def __init__(self, name, h, sem):
        self.name, self.h, self.sem = name, h, sem
        self.count = 0
        self.waited = {}


class FW:
    def __init__(self, nc, stack):
        self.nc = nc
        self.stack = stack
        self.nsem = 0
        self.pe = self._eng("pe", nc.tensor)
        self.act = self._eng("act", nc.scalar)
        self.dve = self._eng("dve", nc.vector)
        self.pool = self._eng("pool", nc.gpsimd)
        self.sp = self._eng("sp", nc.sync)
        self.engs = [self.pe, self.act, self.dve, self.pool, self.sp]
        self.dma_sems = {}
        self.all_dma = []

    def new_sem(self, name):
        self.nsem += 1
        return self.stack.enter_context(self.nc.semaphore(name))

    def _eng(self, name, h):
        return Eng(name, h, self.new_sem("s_" + name))

    def _wait(self, eng, sem, val):
        key = sem.num
        if eng.waited.get(key, 0) >= val:
            return
        eng.waited[key] = val
        eng.h.wait_ge(sem, val)

    def _deps(self, eng, reads, writes, waw=True):
        for b in reads:
            for s, v in b.w.values():
                if s is eng.sem and eng is self.pe:
                    continue
                self._wait(eng, s, v)
        for b in writes:
            for d in ((b.w, b.r) if waw else (b.r,)):
                for s, v in d.values():
                    if s is eng.sem and eng is self.pe:
                        continue
                    self._wait(eng, s, v)

    @staticmethod
    def _rec(d, sem, val):
        k = sem.num
        if k not in d or d[k][1] < val:
            d[k] = (sem, val)

    def _mark(self, sem, val, reads, writes):
        for b in writes:
            b.r = {}
            self._rec(b.w, sem, val)
        for b in reads:
            self._rec(b.r, sem, val)

    def op(self, eng, fn, reads=(), writes=(), inc=True):
        self._deps(eng, reads, writes)
        ins = fn()
        if inc:
            eng.count += 1
            ins.then_inc(eng.sem, 1)
            self._mark(eng.sem, eng.count, reads, writes)
        else:
            self._mark(eng.sem, eng.count + 1, reads, writes)
        return ins

    def dma(self, q, fn, reads=(), writes=(), owner=None, waw=True):
        self._deps(q, reads, writes, waw=waw)
        owner = owner or (writes[0] if writes else reads[0])
        ent = self.dma_sems.get(id(owner))
        if ent is None:
            ent = [self.new_sem("d%d" % self.nsem), 0, owner]
            self.dma_sems[id(owner)] = ent
            self.all_dma.append(ent)
        ent[1] += 16
        ins = fn()
        ins.then_inc(ent[0], 16)
        for b in writes:
            if waw:
                b.r = {}
            self._rec(b.w, ent[0], ent[1])
        for b in reads:
            self._rec(b.r, ent[0], ent[1])
        return ins

    def share(self, owner, *others):
        ent = self.dma_sems.get(id(owner))
        if ent is None:
            ent = [self.new_sem("d%d" % self.nsem), 0, owner]
            self.dma_sems[id(owner)] = ent
            self.all_dma.append(ent)
        for o in others:
            self.dma_sems[id(o)] = ent

    def barrier(self):
        for e in self.engs:
            for x in self.engs:
                if x is not e and x.count > 0:
                    self._wait(e, x.sem, x.count)
            for ent in self.all_dma:
                if ent[1] > 0:
                    self._wait(e, ent[0], ent[1])


def build(nlayers=2, dbg=False, stop_after=None):
    nc = bass.Bass("TRN2", target_bir_lowering=False)

    def din(name, shape, dt=F32):
        return nc.dram_tensor(name, list(shape), dt, kind="ExternalInput").ap()

    def dscr(name, shape, dt, big=False):
        return nc.dram_tensor(name, list(shape), dt, kind="ExternalOutput" if (dbg and not big) else "Internal").ap()

    x_in = din("x", [S, D]); ctx_in = din("ctx", [C, D])
    cc_in = din("cc", [128, KD, 2])
    wada_in = din("w_ada", [2, D, 6 * D]); badac_in = din("b_adac", [128, 2, 48]); bada_in = din("b_ada", [2, 6 * D])
    g1c_in = din("g1c", [128, 2, KD]); g2bc_in = din("g2bc", [2, 128, D]); gfbc_in = din("gfbc", [128, D])
    win_in = din("w_in", [2, D, INW]); wo_in = din("w_o", [2, D, D]); wr_in = din("w_router", [2, D, NE])
    brbc_in = din("b_rbc", [128, 2, NE]); gqk_in = din("gqk", [128, 2, 640]); cs_in = din("cs", [128, 32, 64])
    wdwc_in = din("wdwc", [128, 2, 2, 31]); cvp_in = din("cvp", [128, 2, 2, 3])
    sgp_in = din("sgp", [128, 2, 2, 256]); wsT_in = din("wsT", [128, 2, 4, 128]); bsc_in = din("bsc", [128, 2, 4])
    wg_in = din("w_gate", [2, NE, D, D]); wu_in = din("w_up", [2, NE, D, D]); wd_in = din("w_down", [2, NE, D, D])
    bgu_in = din("bgu", [128, 2, NE, 2, KD]); bd_in = din("b_down", [2, NE, D])
    ident_in = din("ident", [128, 128]); tri_in = din("tri", [128, 128]); eoff_in = din("eoff", [128, NE])
    iota_in = din("iota", [128, NE])
    out_d = nc.dram_tensor("out", [S, D], F32, kind="ExternalOutput").ap()

    QS = dscr("QS", [NTOK, 512], BF16); KS = dscr("KS", [NTOK, 128], BF16)
    US = dscr("US", [16 + S + 32, 256], BF16); USC = dscr("USC", [16 + C + 32, 256], BF16)
    CS = dscr("CS", [NTOK, 256], BF16)
    XM = dscr("XM", [NTOK, D], F32); XS = dscr("XS", [NTOK, D], F32)
    XG = dscr("XG", [NSLOT, D], BF16, big=True); YG = dscr("YG", [NSLOT, D], F32, big=True)
    bQS, bKS, bUS, bUSC, bCS, bXM, bXS, bXG, bYG, bOUT = [Buf(n) for n in
                                                          "QS KS US USC CS XM XS XG YG OUT".split()]
    bIN = Buf("inputs")

    with ExitStack() as top:
        fw = FW(nc, top)
        uid = [0]

        def T(stack, shape, dt, name=None):
            uid[0] += 1
            return stack.enter_context(nc.sbuf_tensor(name or "t%d" % uid[0], list(shape), dt))

        def mk(eng, h):
            def f(name, R=(), W=(), inc=True, **kw):
                return fw.op(eng, lambda: getattr(h, name)(**kw), R, W, inc)
            return f
        dve = mk(fw.dve, nc.vector); act = mk(fw.act, nc.scalar); pool = mk(fw.pool, nc.gpsimd)

        def mm(out, lhsT, rhs, R, W, start=True, stop=True, inc=None):
            return fw.op(fw.pe, lambda: nc.tensor.matmul(out, lhsT=lhsT, rhs=rhs, start=start, stop=stop),
                         R, W, inc=(stop if inc is None else inc))

        def dma(out, in_, R, W, q="sp", waw=True, owner=None):
            h = nc.sync if q == "sp" else nc.gpsimd
            e = fw.sp if q == "sp" else fw.pool
            return fw.dma(e, lambda: h.dma_start(out=out, in_=in_), R, W, owner=owner, waw=waw)

        def dmaT(out, in_, R, W, waw=True, owner=None):
            return fw.dma(fw.sp, lambda: nc.sync.dma_start_transpose(out=out, in_=in_), R, W, owner=owner, waw=waw)

        def rsqrt_col(stack_tiles, src, R, dst, scale, n=1):
            tmp, btmp = stack_tiles
            act("activation", R=R, W=[btmp], out=tmp[:, 0:n], in_=src, func=AF.Sqrt, bias=EPS, scale=scale)
            return tmp, btmp

        bcreg = nc.gpsimd.alloc_register("bcreg")
        nc.gpsimd.reg_mov(bcreg, NSLOT - 1)
        ps = top.enter_context(nc.psum_tensor("ps", [128, 8, 512], F32))
        pb = [Buf("pb%d" % i) for i in range(8)]
        psf = ps[:].rearrange("p b n -> p (b n)")

        bP = Buf("params")
        ident_f = T(top, [128, 128], F32); tri_f = T(top, [128, 128], F32)
        ident_b = T(top, [128, 128], BF16); tri_b = T(top, [128, 128], BF16)
        ones_b = T(top, [128, 128], BF16); ones_f = T(top, [128, 128], F32); onesq = T(top, [128, 128], F32)
        eoff = T(top, [128, NE], F32); iota = T(top, [128, NE], F32)
        cc = T(top, [128, KD, 2], F32); scc = T(top, [128, KD, 2], F32)
        badac = T(top, [128, 2, 48], F32); g1c = T(top, [128, 2, KD], F32)
        brbc = T(top, [128, 2, NE], F32)
        wdwc = T(top, [128, 2, 2, 31], F32); cvp = T(top, [128, 2, 2, 3], F32)
        bsc = T(top, [128, 2, 4], F32)
        wr = T(top, [128, 2, KD, NE], F32)
        zt = T(top, [128, 256], BF16)
        for t_, s_ in ((ident_f, ident_in), (tri_f, tri_in), (eoff, eoff_in), (iota, iota_in), (cc, cc_in),
                       (badac, badac_in), (g1c, g1c_in), (brbc, brbc_in),
                       (wdwc, wdwc_in), (cvp, cvp_in), (bsc, bsc_in)):
            dma(t_[:], s_, R=[bIN], W=[bP], waw=False)
        for l in range(2):
            dma(wr[:, l], wr_in[l].rearrange("(k p) n -> p k n", p=128), R=[bIN], W=[bP], waw=False)
        bC = Buf("consts")
        dve("tensor_copy", R=[bP], W=[bC], out=ident_b[:], in_=ident_f[:])
        dve("tensor_copy", R=[bP], W=[bC], out=tri_b[:], in_=tri_f[:])
        dve("memset", W=[bC], ap=ones_b[:], constant=1.0)
        dve("memset", W=[bC], ap=ones_f[:], constant=1.0)
        dve("memset", W=[bC], ap=onesq[:], constant=1.0 / 256.0)
        dve("memset", W=[bC], ap=zt[:], constant=0.0)
        act("activation", R=[bP], W=[bC], out=scc[:], in_=cc[:], func=AF.Silu)
        dma(US[0:16, :], zt[0:16, :], R=[bC], W=[bUS], waw=False)
        dma(US[16 + S:16 + S + 32, :], zt[0:32, :], R=[bC], W=[bUS], waw=False)
        dma(USC[0:16, :], zt[0:16, :], R=[bC], W=[bUSC], waw=False)
        dma(USC[16 + C:16 + C + 32, :], zt[0:32, :], R=[bC], W=[bUSC], waw=False)

        A1 = T(top, [128, 2, KD], F32); B1 = T(top, [128, 2, KD], F32)
        BC = T(top, [128, 2, 4, D], F32)
        bMOD = Buf("mod")
        sm = T(top, [128, 64], F32); bsm = Buf("sm")
        junk = T(top, [128, D], BF16); bjunk = Buf("junk")

        def compute_mod(l):
            with ExitStack() as ph:
                slab = [T(ph, [128, KD, 512], F32) for _ in range(2)]
                bslab = [Buf("slab%d" % i) for i in range(2)]
                brow = [T(ph, [1, 512], F32) for _ in range(2)]
                bbrow = [Buf("brow%d" % i) for i in range(2)]
                modc = T(ph, [128, 16, 2], F32); bmodc = Buf("modc")
                g2t = T(ph, [128, D], F32); bg2t = Buf("g2t")
                scb = T(ph, [128, KD, 2, 128], F32); bscb = Buf("scb")
                dve("tensor_copy", R=[bC], W=[bscb], out=scb[:], in_=scc[:].unsqueeze(3).to_broadcast([128, KD, 2, 128]))
                dma(g2t[:], g2bc_in[l], R=[bIN], W=[bg2t])
                nwhich = 2 if l == 0 else 1
                for sidx in range(12):
                    sl, bsl = slab[sidx % 2], bslab[sidx % 2]
                    dma(sl[:], wada_in[l, :, sidx * 512:(sidx + 1) * 512].rearrange("(k p) n -> p k n", p=128),
                        R=[bIN], W=[bsl])
                    if sidx < 4:
                        for jj in range(4):
                            j = sidx * 4 + jj
                            for k in range(KD):
                                mm(ps[:, 7, j * 2:j * 2 + 2], sl[:, k, jj * 128:(jj + 1) * 128], scc[:, k, :],
                                   R=[bsl, bC], W=[pb[7]], start=(k == 0), stop=(k == KD - 1))
                        if sidx == 3:
                            dve("tensor_tensor", R=[pb[7], bP], W=[bmodc], out=modc[:],
                                in0=ps[:, 7, 0:32].rearrange("p (j w) -> p j w", w=2),
                                in1=badac[:, l, 0:16].unsqueeze(2).to_broadcast([128, 16, 2]), op=ALU.add)
                            for w in range(2):
                                dve("scalar_tensor_tensor", R=[bmodc, bP], W=[bMOD], out=A1[:, w, :],
                                    in0=modc[:, 8:16, w], scalar=1.0, in1=g1c[:, l, :], op0=ALU.add, op1=ALU.mult)
                                dve("tensor_copy", R=[bmodc], W=[bMOD], out=B1[:, w, :], in_=modc[:, 0:8, w])
                    else:
                        v = (sidx - 4) // 2
                        half = (sidx - 4) % 2
                        br, bbr = brow[sidx % 2], bbrow[sidx % 2]
                        dma(br[:], bada_in[l:l + 1, sidx * 512:(sidx + 1) * 512], R=[bIN], W=[bbr])
                        for w in range(nwhich):
                            bank = 5 + w
                            for k in range(KD):
                                mm(ps[:, bank, :], scb[:, k, w, :], sl[:, k, :], R=[bsl, bscb], W=[pb[bank]],
                                   start=(k == 0), stop=False)
                            mm(ps[:, bank, :], ones_f[0:1, :], br[0:1, :], R=[bbr, bC], W=[pb[bank]],
                               start=False, stop=True)
                            dst = BC[:, w, v, half * 512:(half + 1) * 512]
                            if v == 2:
                                dve("scalar_tensor_tensor", R=[pb[bank], bg2t], W=[bMOD], out=dst,
                                    in0=ps[:, bank, :], scalar=1.0, in1=g2t[:, half * 512:(half + 1) * 512],
                                    op0=ALU.add, op1=ALU.mult)
                            else:
                                act("copy", R=[pb[bank]], W=[bMOD], out=dst, in_=ps[:, bank, :])
                fw.barrier()

        def phase1(l, ph, KT, VA, bKT, bVA):
            WIN = T(ph, [128, KD, INW], BF16); bWIN = Buf("WIN")
            gqk = T(ph, [128, 640], F32); cs = T(ph, [128, 32, 64], F32)
            sgp = T(ph, [128, 2, 256], F32); wsT = T(ph, [128, 4, 128], BF16)
            bP1 = Buf("p1params")
            dma(gqk[:], gqk_in[:, l, :], R=[bIN], W=[bP1], waw=False)
            dma(cs[:], cs_in, R=[bIN], W=[bP1], waw=False)
            dma(sgp[:], sgp_in[:, l], R=[bIN], W=[bP1], waw=False)
            dma(wsT[:], wsT_in[:, l], R=[bIN], W=[bP1], q="pool", waw=False)
            dma(WIN[:], win_in[l].rearrange("(k p) n -> p k n", p=128), R=[bIN], W=[bWIN], q="pool")
            xt = [T(ph, [128, D], F32) for _ in range(2)]; bxt = [Buf("xt%d" % i) for i in range(2)]
            xn = [T(ph, [128, D], BF16) for _ in range(2)]; bxn = [Buf("xn%d" % i) for i in range(2)]
            hT = [T(ph, [128, KD, 128], BF16) for _ in range(2)]; bhT = [Buf("hT%d" % i) for i in range(2)]
            sq = T(ph, [128, 640], F32); bsq = Buf("sq")
            qn = T(ph, [128, 640], F32); bqn = Buf("qn")
            rt = T(ph, [128, 4, 320], F32); brt = Buf("rt")
            qkb = [T(ph, [128, 640], BF16) for _ in range(2)]; bqkb = [Buf("qkb%d" % i) for i in range(2)]
            sig = T(ph, [128, 256], F32); bsig = Buf("sig")
            ub = [T(ph, [128, 256], BF16) for _ in range(2)]; bub = [Buf("ub%d" % i) for i in range(2)]
            zg = T(ph, [128, 512], F32); bzg = Buf("zg")
            vn = T(ph, [128, 256], F32); bvn = Buf("vn")
            vln = T(ph, [128, 256], BF16); bvln = Buf("vln")
            cob = [T(ph, [128, 256], BF16) for _ in range(2)]; bcob = [Buf("cob%d" % i) for i in range(2)]
            st6 = T(ph, [128, 8], F32); bst6 = Buf("st6")
            c1 = T(ph, [128, 32], F32); bc1 = Buf("c1")
            dve("memset", W=[bVA], ap=VA[:, :, :, 64:128], constant=1.0)

            def load(t):
                if l == 0:
                    src = x_in[t * 128:(t + 1) * 128, :] if t < 32 else ctx_in[(t - 32) * 128:(t - 31) * 128, :]
                    dma(xt[t % 2][:], src, R=[bIN], W=[bxt[t % 2]])
                else:
                    dma(xt[t % 2][:], XS[t * 128:(t + 1) * 128, :], R=[bXS], W=[bxt[t % 2]])
            load(0)
            for t in range(34):
                if t + 1 < 34:
                    load(t + 1)
                w = 0 if t < 32 else 1
                X, bX = xt[t % 2], bxt[t % 2]
                XN, bXN = xn[t % 2], bxn[t % 2]
                H, bH = hT[t % 2], bhT[t % 2]
                QB, bQB = qkb[t % 2], bqkb[t % 2]
                act("activation", R=[bX], W=[bjunk, bc1], out=junk[:], in_=X[:], func=AF.Square, accum_out=c1[:, 0:1])
                act("activation", R=[bc1], W=[bc1], out=c1[:, 1:2], in_=c1[:, 0:1], func=AF.Sqrt, bias=EPS, scale=1.0 / D)
                dve("reciprocal", R=[bc1], W=[bc1], out=c1[:, 2:3], in_=c1[:, 1:2])
                dve("tensor_scalar", R=[bX, bc1], W=[bXN], out=XN[:], in0=X[:], scalar1=c1[:, 2:3], scalar2=None, op0=ALU.mult)
                for k in range(KD):
                    mm(ps[:, k // 4, (k % 4) * 128:(k % 4 + 1) * 128], XN[:, k * 128:(k + 1) * 128], ident_b[:],
                       R=[bXN, bC], W=[pb[k // 4]], inc=(k % 4 == 3))
                for k in range(KD):
                    src = ps[:, k // 4, (k % 4) * 128:(k % 4 + 1) * 128]
                    if k % 2 == 0:
                        act("activation", R=[pb[k // 4], bMOD], W=[bH], out=H[:, k, :], in_=src, func=AF.Identity,
                            scale=A1[:, w, k:k + 1], bias=B1[:, w, k:k + 1])
                    else:
                        dve("tensor_scalar", R=[pb[k // 4], bMOD], W=[bH], out=H[:, k, :], in0=src,
                            scalar1=A1[:, w, k:k + 1], scalar2=B1[:, w, k:k + 1], op0=ALU.mult, op1=ALU.add)
                for n in range(4):
                    n0 = n * 512
                    wd_ = min(512, INW - n0)
                    for k in range(KD):
                        mm(ps[:, 2 + n, 0:wd_], H[:, k, :], WIN[:, k, n0:n0 + wd_], R=[bH, bWIN], W=[pb[2 + n]],
                           start=(k == 0), stop=(k == KD - 1))
                P0, P1, P2, P3 = ps[:, 2, :], ps[:, 3, :], ps[:, 4, :], ps[:, 5, :]
                act("activation", R=[pb[2]], W=[bsq], out=sq[:, 0:512], in_=P0, func=AF.Square)
                act("activation", R=[pb[3]], W=[bsq], out=sq[:, 512:640], in_=P1[:, 0:128], func=AF.Square)
                dve("tensor_reduce", R=[bsq], W=[bc1], out=c1[:, 4:14], in_=sq[:].rearrange("p (h d) -> p h d", d=64),
                    axis=AX.X, op=ALU.add)
                act("activation", R=[bc1], W=[bc1], out=c1[:, 14:24], in_=c1[:, 4:14], func=AF.Sqrt, bias=EPS, scale=1.0 / 64)
                dve("reciprocal", R=[bc1], W=[bc1], out=c1[:, 4:14], in_=c1[:, 14:24])
                dve("tensor_tensor", R=[pb[2], bc1], W=[bqn], out=qn[:, 0:512].rearrange("p (h d) -> p h d", d=64),
                    in0=P0.rearrange("p (h d) -> p h d", d=64),
                    in1=c1[:, 4:12].unsqueeze(2).to_broadcast([128, 8, 64]), op=ALU.mult)
                dve("tensor_tensor", R=[pb[3], bc1], W=[bqn], out=qn[:, 512:640].rearrange("p (h d) -> p h d", d=64),
                    in0=P1[:, 0:128].rearrange("p (h d) -> p h d", d=64),
                    in1=c1[:, 12:14].unsqueeze(2).to_broadcast([128, 2, 64]), op=ALU.mult)
                if t < 32:
                    pool("tensor_tensor", R=[bqn, bP1], W=[bqn], out=qn[:], in0=qn[:], in1=gqk[:], op=ALU.mult)
                    q3 = qn[:].rearrange("p (h d) -> p h d", d=64)
                    o3 = QB[:].rearrange("p (h d) -> p h d", d=64)
                    cosb = cs[:, t, 0:32].unsqueeze(1).to_broadcast([128, 10, 32])
                    sinb = cs[:, t, 32:64].unsqueeze(1).to_broadcast([128, 10, 32])
                    r3 = rt[:].rearrange("p a (h d) -> p a h d", d=32)
                    dve("tensor_tensor", R=[bqn, bP1], W=[brt], out=r3[:, 0], in0=q3[:, :, 0:32], in1=cosb, op=ALU.mult)
                    pool("tensor_tensor", R=[bqn, bP1], W=[brt], out=r3[:, 1], in0=q3[:, :, 32:64], in1=sinb, op=ALU.mult)
                    dve("tensor_tensor", R=[bqn, bP1], W=[brt], out=r3[:, 2], in0=q3[:, :, 32:64], in1=cosb, op=ALU.mult)
                    pool("tensor_tensor", R=[bqn, bP1], W=[brt], out=r3[:, 3], in0=q3[:, :, 0:32], in1=sinb, op=ALU.mult)
                    dve("tensor_tensor", R=[brt], W=[bQB], out=o3[:, :, 0:32], in0=r3[:, 0], in1=r3[:, 1], op=ALU.subtract)
                    pool("tensor_tensor", R=[brt], W=[bQB], out=o3[:, :, 32:64], in0=r3[:, 2], in1=r3[:, 3], op=ALU.add)
                else:
                    pool("tensor_tensor", R=[bqn, bP1], W=[bQB], out=QB[:], in0=qn[:], in1=gqk[:], op=ALU.mult)
                for j in range(4):
                    dma(QS[t * 128:(t + 1) * 128, j * 128:(j + 1) * 128].rearrange("p (g d) -> p g d", g=2),
                        QB[:, 0:512].rearrange("p (g j d) -> p j g d", g=2, j=4)[:, j], R=[bQB], W=[bQS], waw=False)
                dma(KS[t * 128:(t + 1) * 128, :], QB[:, 512:640], R=[bQB], W=[bKS], waw=False)
                act("copy", R=[pb[3]], W=[bVA], out=VA[:, t, :, 0:64], in_=P1[:, 128:256].rearrange("p (g d) -> p g d", d=64))
                UB, bUB = ub[t % 2], bub[t % 2]
                act("activation", R=[pb[4]], W=[bsig], out=sig[:], in_=P2[:, 0:256], func=AF.Sigmoid)
                dve("tensor_tensor", R=[pb[3], bsig], W=[bUB], out=UB[:], in0=P1[:, 256:512], in1=sig[:], op=ALU.mult)
                if t < 32:
                    dma(US[16 + t * 128:16 + (t + 1) * 128, :], UB[:], R=[bUB], W=[bUS], waw=False)
                else:
                    dma(USC[16 + (t - 32) * 128:16 + (t - 31) * 128, :], UB[:], R=[bUB], W=[bUSC], waw=False)
                act("activation", R=[pb[4], pb[5]], W=[bzg], out=zg[:], in_=psf[:, 4 * 512 + 256:5 * 512 + 256], func=AF.Gelu)
                dve("bn_stats", R=[bzg], W=[bst6], out=st6[:, 0:6], in_=zg[:, 256:512])
                dve("bn_aggr", R=[bst6], W=[bst6], out=st6[:, 6:8], in_=st6[:, 0:6])
                act("activation", R=[bst6], W=[bc1], out=c1[:, 24:25], in_=st6[:, 7:8], func=AF.Sqrt, bias=EPS, scale=1.0)
                dve("reciprocal", R=[bc1], W=[bc1], out=c1[:, 25:26], in_=c1[:, 24:25])
                dve("tensor_scalar", R=[bzg, bst6, bc1], W=[bvn], out=vn[:], in0=zg[:, 256:512], scalar1=st6[:, 6:7],
                    scalar2=c1[:, 25:26], op0=ALU.subtract, op1=ALU.mult)
                pool("tensor_tensor", R=[bvn, bP1], W=[bvn], out=vn[:], in0=vn[:], in1=sgp[:, 0, :], op=ALU.mult)
                pool("tensor_tensor", R=[bvn, bP1], W=[bvln], out=vln[:], in0=vn[:], in1=sgp[:, 1, :], op=ALU.add)
                for h in range(4):
                    mm(ps[:, 6, h * 64:(h + 1) * 64], wsT[:, h, :], vln[:, h * 64:(h + 1) * 64], R=[bvln, bP1], W=[pb[6]],
                       inc=(h == 3))
                CO, bCO = cob[t % 2], bcob[t % 2]
                for h in range(4):
                    dve("scalar_tensor_tensor", R=[pb[6], bzg, bP], W=[bCO], out=CO[:, h * 64:(h + 1) * 64],
                        in0=ps[:, 6, h * 64:(h + 1) * 64], scalar=bsc[:, l, h:h + 1], in1=zg[:, h * 64:(h + 1) * 64],
                        op0=ALU.add, op1=ALU.mult)
                dma(CS[t * 128:(t + 1) * 128, :], CO[:], R=[bCO], W=[bCS], waw=False)
            for i in range(34):
                dmaT(KT[:, i * 128:(i + 1) * 128], KS[i * 128:(i + 1) * 128, :], R=[bKS], W=[bKT], waw=False)

        def phase2(l, ph, KT, VA, bKT, bVA, DEST, W4, bRT):
            WO = T(ph, [128, KD, D], BF16); bWO = Buf("WO")
            dma(WO[:], wo_in[l].rearrange("(k p) n -> p k n", p=128), R=[bIN], W=[bWO], q="pool")
            DG = T(ph, [128, 2, 31, 128], BF16); bDG = Buf("DG")
            for c_ in range(2):
                for k in range(31):
                    e_ = dve if (k % 2 == 0) else pool
                    e_("tensor_scalar", R=[bC, bP], W=[bDG], out=DG[:, c_, k, :], in0=ident_f[:],
                       scalar1=wdwc[:, l, c_, k:k + 1], scalar2=None, op0=ALU.mult)
            QTb = [T(ph, [128, 4, 512], BF16) for _ in range(2)]; bQTb = [Buf("QTb%d" % i) for i in range(2)]
            UTb = [T(ph, [128, 2, 544], BF16) for _ in range(2)]; bUTb = [Buf("UTb%d" % i) for i in range(2)]
            CTb = [T(ph, [128, 2, 512], BF16) for _ in range(2)]; bCTb = [Buf("CTb%d" % i) for i in range(2)]
            cv = T(ph, [128, 2, 512], F32); bcv = Buf("cv")
            sqv = T(ph, [128, 2, 512], F32); bsqv = Buf("sqv")
            msq = T(ph, [128, 512], F32); bmsq = Buf("msq")
            rstd = T(ph, [128, 512], F32); brstd = Buf("rstd")
            CVb = T(ph, [128, 2, 512], BF16); bCVb = Buf("CVb")
            PT = [T(ph, [128, 2, 512], BF16) for _ in range(2)]; bPT = [Buf("PT%d" % i) for i in range(2)]
            AT = T(ph, [128, 4, 512], BF16); bAT = Buf("AT")
            rec = T(ph, [64, 512], F32); brec = Buf("rec")
            xr = [T(ph, [128, D], F32) for _ in range(2)]; bxr = [Buf("xr%d" % i) for i in range(2)]
            xm = [T(ph, [128, D], F32) for _ in range(2)]; bxm = [Buf("xm%d" % i) for i in range(2)]
            tmp = T(ph, [128, D], F32); btmp = Buf("tmp")
            h2 = T(ph, [128, D], F32); bh2 = Buf("h2")
            h2b = [T(ph, [128, D], BF16) for _ in range(2)]; bh2b = [Buf("h2b%d" % i) for i in range(2)]
            h2T = T(ph, [128, KD, 128], F32); bh2T = Buf("h2T")
            c1 = T(ph, [128, 8], F32); bc1 = Buf("c1b")
            lg = T(ph, [128, NE], F32); blg = Buf("lg")
            mx8 = T(ph, [128, 8], F32); bmx8 = Buf("mx8")
            ix8 = T(ph, [128, 8], U32); bix8 = Buf("ix8")
            ixf = T(ph, [128, 8], F32); bixf = Buf("ixf")
            e4 = T(ph, [128, 8], F32); be4 = Buf("e4")
            mask = T(ph, [128, NE], BF16); bmask = Buf("mask")
            cnt = T(ph, [128, NE], F32); bcnt = Buf("cnt")
            posf = T(ph, [128, NE], F32); bposf = Buf("posf")
            val = T(ph, [128, NE], F32); bval = Buf("val")
            oh = T(ph, [128, 4, NE], F32); boh = Buf("oh")
            dk = T(ph, [128, 8], F32); bdk = Buf("dk")
            dve("memset", W=[bcnt], ap=cnt[:], constant=0.0)

            blocks = [(b * 512, 512, list(range(34)), 0) for b in range(8)]
            if l == 0:
                blocks.append((S, 256, [32, 33], 1))

            def loads(bi):
                tok0, nt, kcs, w = blocks[bi]
                Q, bQ = QTb[bi % 2], bQTb[bi % 2]
                U, bU = UTb[bi % 2], bUTb[bi % 2]
                Cc, bCc = CTb[bi % 2], bCTb[bi % 2]
                for j in range(4):
                    dmaT(Q[:, j, 0:nt], QS[tok0:tok0 + nt, j * 128:(j + 1) * 128], R=[bQS], W=[bQ], waw=(j == 0))
                for c_ in range(2):
                    if w == 0:
                        dmaT(U[:, c_, 0:nt + 32], US[tok0:tok0 + nt + 32, c_ * 128:(c_ + 1) * 128], R=[bUS], W=[bU], waw=(c_ == 0))
                    else:
                        dmaT(U[:, c_, 0:nt + 32], USC[0:nt + 32, c_ * 128:(c_ + 1) * 128], R=[bUSC], W=[bU], waw=(c_ == 0))
                    dmaT(Cc[:, c_, 0:nt], CS[tok0:tok0 + nt, c_ * 128:(c_ + 1) * 128], R=[bCS], W=[bCc], waw=(c_ == 0))

            loads(0)
            for bi, (tok0, nt, kcs, w) in enumerate(blocks):
                if bi + 1 < len(blocks):
                    loads(bi + 1)
                Q, bQ = QTb[bi % 2], bQTb[bi % 2]
                U, bU = UTb[bi % 2], bUTb[bi % 2]
                Cc, bCc = CTb[bi % 2], bCTb[bi % 2]
                for c_ in range(2):
                    for k in range(31):
                        mm(ps[:, c_, 0:nt], DG[:, c_, k, :], U[:, c_, 1 + k:1 + k + nt], R=[bDG, bU], W=[pb[c_]],
                           start=(k == 0), stop=(k == 30))
                    act("activation", R=[pb[c_], bP], W=[bcv], out=cv[:, c_, 0:nt], in_=ps[:, c_, 0:nt], func=AF.Identity,
                        bias=cvp[:, l, c_, 0:1], scale=1.0)
                    act("activation", R=[bcv], W=[bsqv], out=sqv[:, c_, 0:nt], in_=cv[:, c_, 0:nt], func=AF.Square)
                for c_ in range(2):
                    mm(ps[:, 2, 0:nt], onesq[:], cv[:, c_, 0:nt], R=[bcv, bC], W=[pb[2]], start=(c_ == 0), stop=(c_ == 1))
                for c_ in range(2):
                    mm(ps[:, 0, 0:nt], onesq[:], sqv[:, c_, 0:nt], R=[bsqv, bC], W=[pb[0]], start=(c_ == 0), stop=(c_ == 1))
                act("activation", R=[pb[2]], W=[bmsq], out=msq[:, 0:nt], in_=ps[:, 2, 0:nt], func=AF.Square)
                dve("tensor_tensor", R=[pb[0], bmsq], W=[bmsq], out=msq[:, 0:nt], in0=ps[:, 0, 0:nt], in1=msq[:, 0:nt], op=ALU.subtract)
                act("activation", R=[bmsq], W=[brstd], out=rstd[:, 0:nt], in_=msq[:, 0:nt], func=AF.Sqrt, bias=EPS, scale=1.0)
                dve("reciprocal", R=[brstd], W=[brstd], out=rstd[:, 0:nt], in_=rstd[:, 0:nt])
                for c_ in range(2):
                    dve("tensor_tensor", R=[bcv, pb[2]], W=[bcv], out=cv[:, c_, 0:nt], in0=cv[:, c_, 0:nt], in1=ps[:, 2, 0:nt], op=ALU.subtract)
                    pool("tensor_tensor", R=[bcv, brstd], W=[bcv], out=cv[:, c_, 0:nt], in0=cv[:, c_, 0:nt], in1=rstd[:, 0:nt], op=ALU.mult)
                    act("activation", R=[bcv, bP], W=[bCVb], out=CVb[:, c_, 0:nt], in_=cv[:, c_, 0:nt], func=AF.Silu,
                        scale=cvp[:, l, c_, 1:2], bias=cvp[:, l, c_, 2:3])
                steps = [(h, i) for h in range(8) for i in range(len(kcs))]

                npairs = len(steps) // 2

                def score_pair(p):
                    for u_ in range(2):
                        h, i = steps[2 * p + u_]
                        g, j = h // 4, h % 4
                        kc = kcs[i]
                        bk = 2 * (p % 2) + u_
                        mm(ps[:, bk, 0:nt], KT[g * 64:(g + 1) * 64, kc * 128:(kc + 1) * 128], Q[g * 64:(g + 1) * 64, j, 0:nt],
                           R=[bKT, bQ], W=[pb[bk]])
                score_pair(0)
                if npairs > 1:
                    score_pair(1)
                for p in range(npairs):
                    q_ = p % 2
                    P2, bP2 = PT[q_], bPT[q_]
                    act("activation", R=[pb[2 * q_], pb[2 * q_ + 1]], W=[bP2], out=P2[:, :, 0:nt], in_=ps[:, 2 * q_:2 * q_ + 2, 0:nt],
                        func=AF.Exp, scale=0.125)
                    for u_ in range(2):
                        h, i = steps[2 * p + u_]
                        g = h // 4
                        kc = kcs[i]
                        ob = 4 + (h % 2)
                        mm(ps[:, ob, 0:nt], VA[:, kc, g, :], P2[:, u_, 0:nt], R=[bVA, bP2], W=[pb[ob]],
                           start=(i == 0), stop=(i == len(kcs) - 1), inc=True)
                    if p + 2 < npairs:
                        score_pair(p + 2)
                    h, i = steps[2 * p + 1]
                    if i == len(kcs) - 1:
                        ob = 4 + (h % 2)
                        dve("reciprocal", R=[pb[ob]], W=[brec], out=rec[:, 0:nt], in_=ps[64:128, ob, 0:nt])
                        dve("tensor_tensor", R=[pb[ob], brec], W=[bAT], out=AT[(h % 2) * 64:(h % 2) * 64 + 64, h // 2, 0:nt],
                            in0=ps[0:64, ob, 0:nt], in1=rec[:, 0:nt], op=ALU.mult)
                for s_ in range(nt // 128):
                    t = (tok0 // 128) + s_
                    cs_ = slice(s_ * 128, (s_ + 1) * 128)
                    XR, bXR = xr[t % 2], bxr[t % 2]
                    XMt, bXMt = xm[t % 2], bxm[t % 2]
                    H2B, bH2B = h2b[t % 2], bh2b[t % 2]
                    if l == 0:
                        src = x_in[t * 128:(t + 1) * 128, :] if t < 32 else ctx_in[(t - 32) * 128:(t - 31) * 128, :]
                        dma(XR[:], src, R=[bIN], W=[bXR])
                    else:
                        dma(XR[:], XS[t * 128:(t + 1) * 128, :], R=[bXS], W=[bXR])
                    for n in range(2):
                        for kk in range(KD):
                            if kk < 4:
                                lh, bl = AT[:, kk, cs_], bAT
                            elif kk < 6:
                                lh, bl = CVb[:, kk - 4, cs_], bCVb
                            else:
                                lh, bl = Cc[:, kk - 6, cs_], bCc
                            mm(ps[:, 6 + n, :], lh, WO[:, kk, n * 512:(n + 1) * 512], R=[bl, bWO], W=[pb[6 + n]],
                               start=(kk == 0), stop=(kk == KD - 1))
                    dve("tensor_tensor", R=[pb[6], pb[7], bMOD], W=[btmp], out=tmp[:], in0=psf[:, 6 * 512:8 * 512], in1=BC[:, w, 0, :], op=ALU.mult)
                    pool("tensor_tensor", R=[btmp, bXR], W=[bXMt], out=XMt[:], in0=tmp[:], in1=XR[:], op=ALU.add)
                    dma(XM[t * 128:(t + 1) * 128, :], XMt[:], R=[bXMt], W=[bXM], waw=False)
                    act("activation", R=[bXMt], W=[bjunk, bc1], out=junk[:], in_=XMt[:], func=AF.Square, accum_out=c1[:, 0:1])
                    act("activation", R=[bc1], W=[bc1], out=c1[:, 1:2], in_=c1[:, 0:1], func=AF.Sqrt, bias=EPS, scale=1.0 / D)
                    dve("reciprocal", R=[bc1], W=[bc1], out=c1[:, 2:3], in_=c1[:, 1:2])
                    dve("scalar_tensor_tensor", R=[bXMt, bc1, bMOD], W=[btmp], out=tmp[:], in0=XMt[:], scalar=c1[:, 2:3], in1=BC[:, w, 2, :],
                        op0=ALU.mult, op1=ALU.mult)
                    pool("tensor_tensor", R=[btmp, bMOD], W=[bh2], out=h2[:], in0=tmp[:], in1=BC[:, w, 1, :], op=ALU.add)
                    act("copy", R=[bh2], W=[bH2B], out=H2B[:], in_=h2[:])
                    for k in range(KD):
                        mm(ps[:, k // 4, (k % 4) * 128:(k % 4 + 1) * 128], h2[:, k * 128:(k + 1) * 128], ident_f[:],
                           R=[bh2, bP], W=[pb[k // 4]], inc=(k % 4 == 3))
                    act("copy", R=[pb[0]], W=[bh2T], out=h2T[:, 0:4, :], in_=ps[:, 0, :].rearrange("p (k n) -> p k n", n=128))
                    dve("tensor_copy", R=[pb[1]], W=[bh2T], out=h2T[:, 4:8, :], in_=ps[:, 1, :].rearrange("p (k n) -> p k n", n=128))
                    for k in range(KD):
                        mm(ps[:, 4, 0:NE], h2T[:, k, :], wr[:, l, k, :], R=[bh2T, bP], W=[pb[4]], start=(k == 0), stop=(k == KD - 1))
                    dve("tensor_tensor", R=[pb[4], bP], W=[blg], out=lg[:], in0=ps[:, 4, 0:NE], in1=brbc[:, l, :], op=ALU.add)
                    dve("max", R=[blg], W=[bmx8], out=mx8[:], in_=lg[:])
                    dve("max_index", R=[blg, bmx8], W=[bix8], out=ix8[:], in_max=mx8[:], in_values=lg[:])
                    dve("tensor_copy", R=[bix8], W=[bixf], out=ixf[:], in_=ix8[:])
                    dve("tensor_scalar", R=[bmx8], W=[be4], out=e4[:, 4:5], in0=mx8[:, 0:1], scalar1=-1.0, scalar2=None, op0=ALU.mult)
                    act("activation", R=[bmx8, be4], W=[be4], out=e4[:, 0:4], in_=mx8[:, 0:4], func=AF.Exp, bias=e4[:, 4:5], scale=1.0,
                        accum_out=e4[:, 5:6])
                    dve("reciprocal", R=[be4], W=[be4], out=e4[:, 6:7], in_=e4[:, 5:6])
                    dve("tensor_scalar", R=[blg, bmx8], W=[bmask], out=mask[:], in0=lg[:], scalar1=mx8[:, 3:4], scalar2=None, op0=ALU.is_ge)
                    mm(ps[:, 4, 32:64], tri_b[:], mask[:], R=[bmask, bC], W=[pb[4]])
                    mm(ps[:, 4, 64:96], ones_b[:], mask[:], R=[bmask, bC], W=[pb[4]])
                    dve("tensor_tensor", R=[pb[4], bcnt], W=[bposf], out=posf[:], in0=ps[:, 4, 32:64], in1=cnt[:], op=ALU.add)
                    dve("tensor_tensor", R=[pb[4], bcnt], W=[bcnt], out=cnt[:], in0=ps[:, 4, 64:96], in1=cnt[:], op=ALU.add)
                    dve("tensor_scalar", R=[bposf], W=[bval], out=val[:], in0=posf[:], scalar1=float(CAP), scalar2=None, op0=ALU.is_lt)
                    dve("tensor_tensor", R=[bposf, bP], W=[bposf], out=posf[:], in0=posf[:], in1=eoff[:], op=ALU.add)
                    dve("tensor_scalar", R=[bposf], W=[bposf], out=posf[:], in0=posf[:], scalar1=-BIG, scalar2=None, op0=ALU.add)
                    dve("tensor_tensor", R=[bposf, bval], W=[bposf], out=posf[:], in0=posf[:], in1=val[:], op=ALU.mult)
                    dve("tensor_scalar", R=[bposf], W=[bposf], out=posf[:], in0=posf[:], scalar1=BIG, scalar2=None, op0=ALU.add)
                    for k in range(4):
                        dve("tensor_scalar", R=[bixf, bP], W=[boh], out=oh[:, k, :], in0=iota[:], scalar1=ixf[:, k:k + 1], scalar2=None,
                            op0=ALU.is_equal)
                    dve("tensor_tensor", R=[boh, bposf], W=[boh], out=oh[:], in0=oh[:], in1=posf[:].unsqueeze(1).to_broadcast([128, 4, NE]),
                        op=ALU.mult)
                    dve("tensor_reduce", R=[boh], W=[bdk], out=dk[:, 0:4], in_=oh[:], axis=AX.X, op=ALU.add)
                    dve("tensor_copy", R=[bdk], W=[bRT], out=DEST[:, t, :], in_=dk[:, 0:4])
                    dve("tensor_scalar", R=[bdk], W=[bdk], out=dk[:, 4:8], in0=dk[:, 0:4], scalar1=float(NSLOT), scalar2=None, op0=ALU.is_lt)
                    dve("tensor_scalar", R=[be4], W=[be4], out=e4[:, 0:4], in0=e4[:, 0:4], scalar1=e4[:, 6:7], scalar2=None, op0=ALU.mult)
                    dve("tensor_tensor", R=[be4, bdk], W=[bRT], out=W4[:, t, :], in0=e4[:, 0:4], in1=dk[:, 4:8], op=ALU.mult)
                    for k in range(4):
                        fw.dma(fw.pool, lambda k=k: nc.gpsimd.indirect_dma_start(
                            out=XG, out_offset=bass.IndirectOffsetOnAxis(ap=DEST[:, t, k:k + 1], axis=0),
                            in_=H2B[:], in_offset=None, bounds_check=bcreg, oob_is_err=False),
                            reads=[bH2B, bRT], writes=[bXG], waw=False)

        def phase3(l, ph):
            NB = CAP // 512
            WB = [T(ph, [128, KD, D], BF16) for _ in range(4)]; bWB = [Buf("WB%d" % i) for i in range(4)]
            BD = [T(ph, [1, D], BF16) for _ in range(2)]; bBD = [Buf("BD%d" % i) for i in range(2)]
            bgu = T(ph, [128, NE, 2, KD], F32); bBGU = Buf("bgu")
            dma(bgu[:], bgu_in[:, l], R=[bIN], W=[bBGU])
            xT = [T(ph, [128, KD, 512], BF16) for _ in range(2)]; bxT = [Buf("xT%d" % i) for i in range(2)]
            aT = [T(ph, [128, KD, 512], BF16) for _ in range(2)]; baT = [Buf("aT%d" % i) for i in range(2)]
            gb = [T(ph, [128, 512], F32) for _ in range(2)]; bgb = [Buf("gb%d" % i) for i in range(2)]
            sg = [T(ph, [128, 512], F32) for _ in range(2)]; bsg = [Buf("sg%d" % i) for i in range(2)]
            ubt = [T(ph, [128, 512], F32) for _ in range(2)]; bubt = [Buf("ubt%d" % i) for i in range(2)]
            yt = [T(ph, [128, D], F32) for _ in range(2)]; byt = [Buf("yt%d" % i) for i in range(2)]
            wsrc = (wg_in, wu_in, wd_in)

            def mload(m):
                e, j = m // 3, m % 3
                dma(WB[m % 4][:], wsrc[j][l, e].rearrange("(k p) n -> p k n", p=128), R=[bIN], W=[bWB[m % 4]], q="pool")
                if j == 2:
                    dma(BD[e % 2][:], bd_in[l, e:e + 1, :], R=[bIN], W=[bBD[e % 2]], q="pool")

            items = [(e, b) for e in range(NE) for b in range(NB)]

            def xload(ii):
                e, b = items[ii]
                r0 = e * CAP + b * 512
                for k in range(KD):
                    dmaT(xT[ii % 2][:, k, :], XG[r0:r0 + 512, k * 128:(k + 1) * 128], R=[bXG], W=[bxT[ii % 2]], waw=(k == 0))
            nload = [0]

            def prefetch(upto):
                while nload[0] <= upto and nload[0] < 3 * NE:
                    mload(nload[0])
                    nload[0] += 1
            prefetch(2)
            xload(0)
            fcount = 0
            ycount = 0
            for ii, (e, b) in enumerate(items):
                WGe, bWGe = WB[(3 * e) % 4], bWB[(3 * e) % 4]
                WUe, bWUe = WB[(3 * e + 1) % 4], bWB[(3 * e + 1) % 4]
                WDe, bWDe = WB[(3 * e + 2) % 4], bWB[(3 * e + 2) % 4]
                if b == 0:
                    prefetch(3 * e + 3)
                if ii + 1 < len(items):
                    xload(ii + 1)
                X, bX = xT[ii % 2], bxT[ii % 2]
                A, bA = aT[ii % 2], baT[ii % 2]
                for f in range(KD):
                    pg = (fcount % 2) * 2
                    fcount += 1
                    for k in range(KD):
                        mm(ps[:, pg, :], WGe[:, k, f * 128:(f + 1) * 128], X[:, k, :], R=[bWGe, bX], W=[pb[pg]],
                           start=(k == 0), stop=(k == KD - 1))
                    for k in range(KD):
                        mm(ps[:, pg + 1, :], WUe[:, k, f * 128:(f + 1) * 128], X[:, k, :], R=[bWUe, bX], W=[pb[pg + 1]],
                           start=(k == 0), stop=(k == KD - 1))
                    G, bG = gb[f % 2], bgb[f % 2]
                    SG, bSG = sg[f % 2], bsg[f % 2]
                    UU, bUU = ubt[f % 2], bubt[f % 2]
                    dve("tensor_scalar", R=[pb[pg], bBGU], W=[bG], out=G[:], in0=ps[:, pg, :], scalar1=bgu[:, e, 0, f:f + 1], scalar2=7.0,
                        op0=ALU.add, op1=ALU.min)
                    act("activation", R=[bG], W=[bSG], out=SG[:], in_=G[:], func=AF.Sigmoid, scale=1.702)
                    dve("tensor_scalar", R=[pb[pg + 1], bBGU], W=[bUU], out=UU[:], in0=ps[:, pg + 1, :], scalar1=bgu[:, e, 1, f:f + 1],
                        scalar2=7.0, op0=ALU.add, op1=ALU.min)
                    dve("tensor_scalar", R=[bUU], W=[bUU], out=UU[:], in0=UU[:], scalar1=-7.0, scalar2=1.0, op0=ALU.max, op1=ALU.add)
                    dve("tensor_tensor", R=[bG, bSG], W=[bSG], out=SG[:], in0=G[:], in1=SG[:], op=ALU.mult)
                    dve("tensor_tensor", R=[bUU, bSG], W=[bA], out=A[:, f, :], in0=UU[:], in1=SG[:], op=ALU.mult)
                for s4 in range(4):
                    Y, bY = yt[ycount % 2], byt[ycount % 2]
                    pbase = 4 + (ycount % 2) * 2
                    ycount += 1
                    for n in range(2):
                        for f in range(KD):
                            mm(ps[:, pbase + n, :], A[:, f, s4 * 128:(s4 + 1) * 128], WDe[:, f, n * 512:(n + 1) * 512],
                               R=[bA, bWDe], W=[pb[pbase + n]], start=(f == 0), stop=False)
                        mm(ps[:, pbase + n, :], ones_b[0:1, :], BD[e % 2][0:1, n * 512:(n + 1) * 512], R=[bBD[e % 2], bC], W=[pb[pbase + n]],
                           start=False, stop=True)
                    act("copy", R=[pb[pbase]], W=[bY], out=Y[:, 0:512], in_=ps[:, pbase, :])
                    dve("tensor_copy", R=[pb[pbase + 1]], W=[bY], out=Y[:, 512:1024], in_=ps[:, pbase + 1, :])
                    r0 = e * CAP + b * 512 + s4 * 128
                    dma(YG[r0:r0 + 128, :], Y[:], R=[bY], W=[bYG], waw=False)

        def phase4(l, ph, DEST, W4, bRT):
            last = (l == nlayers - 1)
            Y4 = [T(ph, [128, 4, D], F32) for _ in range(2)]; bY4 = [Buf("Y4%d" % i) for i in range(2)]
            xmt = [T(ph, [128, D], F32) for _ in range(2)]; bxmt = [Buf("xmt%d" % i) for i in range(2)]
            acc = T(ph, [128, D], F32); bacc = Buf("acc")
            xo = [T(ph, [128, D], F32) for _ in range(2)]; bxo = [Buf("xo%d" % i) for i in range(2)]
            c1 = T(ph, [128, 8], F32); bc1 = Buf("c1c")
            gfbc = T(ph, [128, D], F32); bGF = Buf("gfbc")
            if last:
                dma(gfbc[:], gfbc_in, R=[bIN], W=[bGF])
            for i in range(2):
                pool("memset", W=[bY4[i]], ap=Y4[i][:], constant=0.0)
            ntile = 32 if last else 34

            def loads(t):
                dma(xmt[t % 2][:], XM[t * 128:(t + 1) * 128, :], R=[bXM], W=[bxmt[t % 2]])
                for k in range(4):
                    fw.dma(fw.pool, lambda k=k: nc.gpsimd.indirect_dma_start(
                        out=Y4[t % 2][:, k, :], out_offset=None, in_=YG,
                        in_offset=bass.IndirectOffsetOnAxis(ap=DEST[:, t, k:k + 1], axis=0),
                        bounds_check=bcreg, oob_is_err=False),
                        reads=[bYG, bRT], writes=[bY4[t % 2]], waw=(k == 0))
            loads(0)
            for t in range(ntile):
                if t + 1 < ntile:
                    loads(t + 1)
                w = 0 if t < 32 else 1
                Y, bY = Y4[t % 2], bY4[t % 2]
                XMt, bXMt = xmt[t % 2], bxmt[t % 2]
                XO, bXO = xo[t % 2], bxo[t % 2]
                dve("tensor_scalar", R=[bY, bRT], W=[bacc], out=acc[:], in0=Y[:, 0, :], scalar1=W4[:, t, 0:1], scalar2=None, op0=ALU.mult)
                for k in range(1, 4):
                    dve("scalar_tensor_tensor", R=[bY, bRT, bacc], W=[bacc], out=acc[:], in0=Y[:, k, :], scalar=W4[:, t, k:k + 1], in1=acc[:],
                       op0=ALU.mult, op1=ALU.add)
                pool("tensor_tensor", R=[bacc, bMOD], W=[bacc], out=acc[:], in0=acc[:], in1=BC[:, w, 3, :], op=ALU.mult)
                dve("tensor_tensor", R=[bacc, bXMt], W=[bXO], out=XO[:], in0=acc[:], in1=XMt[:], op=ALU.add)
                if not last:
                    dma(XS[t * 128:(t + 1) * 128, :], XO[:], R=[bXO], W=[bXS], waw=False)
                else:
                    act("activation", R=[bXO], W=[bjunk, bc1], out=junk[:], in_=XO[:], func=AF.Square, accum_out=c1[:, 0:1])
                    act("activation", R=[bc1], W=[bc1], out=c1[:, 1:2], in_=c1[:, 0:1], func=AF.Sqrt, bias=EPS, scale=1.0 / D)
                    dve("reciprocal", R=[bc1], W=[bc1], out=c1[:, 2:3], in_=c1[:, 1:2])
                    dve("scalar_tensor_tensor", R=[bXO, bc1, bGF], W=[bXO], out=XO[:], in0=XO[:], scalar=c1[:, 2:3], in1=gfbc[:],
                        op0=ALU.mult, op1=ALU.mult)
                    dma(out_d[t * 128:(t + 1) * 128, :], XO[:], R=[bXO], W=[bOUT], waw=False)

        done = False
        for l in range(nlayers):
            compute_mod(l)
            with ExitStack() as lay:
                DEST = T(lay, [128, 34, 4], I32); W4 = T(lay, [128, 34, 4], F32); bRT = Buf("route")
                with ExitStack() as att:
                    KT = T(att, [128, NTOK], BF16); bKT = Buf("KT")
                    VA = T(att, [128, 34, 2, 128], BF16); bVA = Buf("VA")
                    with ExitStack() as ph:
                        phase1(l, ph, KT, VA, bKT, bVA)
                        fw.barrier()
                    if stop_after == (l, 1):
                        done = True
                    if not done:
                        with ExitStack() as ph:
                            phase2(l, ph, KT, VA, bKT, bVA, DEST, W4, bRT)
                            fw.barrier()
                        if stop_after == (l, 2):
                            done = True
                if not done:
                    with ExitStack() as ph:
                        phase3(l, ph)
                        fw.barrier()
                    if stop_after == (l, 3):
                        done = True
                if not done:
                    with ExitStack() as ph:
                        phase4(l, ph, DEST, W4, bRT)
                        fw.barrier()
            if done:
                break
        fw.barrier()
    return nc


def _rope_tables():
    pos = np.arange(S)
    r = (pos // 64).astype(np.float32)
    col = (pos % 64).astype(np.float32)
    inv = (10000.0 ** (-np.arange(0, 32, 2, dtype=np.float32) / 32.0)).astype(np.float32)
    ang = np.concatenate([r[:, None] * inv[None, :], col[:, None] * inv[None, :]], axis=-1).astype(np.float32)
    tab = np.concatenate([np.cos(ang), np.sin(ang)], axis=-1).astype(np.float32)
    return np.ascontiguousarray(tab.reshape(32, 128, 64).transpose(1, 0, 2))


def _col(v):
    v = np.asarray(v, dtype=np.float32)
    lead = v.shape[:-1]
    n = v.shape[-1] // 128
    a = v.reshape(*lead, n, 128)
    return np.ascontiguousarray(np.moveaxis(a, -1, 0))


def make_in_maps(inp, cores):
    f = lambda a: np.ascontiguousarray(np.asarray(a, dtype=np.float32))
    bc = lambda v: np.ascontiguousarray(np.broadcast_to(np.asarray(v, np.float32), (128,) + np.asarray(v).shape))
    shared = {
        "w_ada": f(inp["w_ada"]), "b_ada": f(inp["b_ada"]),
        "b_adac": _col(inp["b_ada"]),
        "g1c": _col(inp["g_norm1"]),
        "g2bc": np.ascontiguousarray(np.broadcast_to(f(inp["g_norm2"])[:, None, :], (2, 128, D))),
        "gfbc": bc(inp["g_final"]),
        "w_in": f(inp["w_in"]), "w_o": f(inp["w_o"]), "w_router": f(inp["w_router"]),
        "b_rbc": bc(inp["b_router"]),
        "gqk": bc(np.concatenate([np.tile(f(inp["g_q"]), (1, 8)), np.tile(f(inp["g_k"]), (1, 2))], axis=1)),
        "cs": _rope_tables(),
        "wdwc": np.ascontiguousarray(_col(inp["w_dw"])),
        "cvp": np.ascontiguousarray(np.stack([_col(inp["b_dw"]), _col(inp["g_conv_ln"]), _col(inp["b_conv_ln"])], axis=-1)),
        "sgp": bc(np.stack([f(inp["g_sgu_ln"]), f(inp["b_sgu_ln"])], axis=1)),
        "wsT": np.ascontiguousarray(f(inp["w_s"]).transpose(3, 0, 1, 2)),
        "bsc": np.ascontiguousarray(f(inp["b_s"]).transpose(2, 0, 1)),
        "w_gate": f(inp["w_gate"]), "w_up": f(inp["w_up"]), "w_down": f(inp["w_down"]),
        "bgu": np.ascontiguousarray(np.stack([_col(inp["b_gate"]), _col(inp["b_up"])], axis=3)),
        "b_down": f(inp["b_down"]),
        "ident": np.eye(128, dtype=np.float32),
        "tri": np.triu(np.ones((128, 128), np.float32), k=1),
        "eoff": bc(np.arange(NE, dtype=np.float32) * CAP),
        "iota": bc(np.arange(NE, dtype=np.float32)),
    }
    shared["wdwc"] = np.ascontiguousarray(shared["wdwc"].transpose(0, 1, 3, 2))
    maps = []
    for b in cores:
        m = dict(shared)
        m["x"] = f(inp["x"][b]); m["ctx"] = f(inp["ctx"][b])
        m["cc"] = np.ascontiguousarray(np.stack([_col(inp["c"][b]), _col(inp["c_ctx"])], axis=-1))
        maps.append(m)
    return maps


_NC = None


def kernel(**inputs):
    global _NC
    if _NC is None:
        _NC = build()
    maps = make_in_maps(inputs, list(range(8)))
    res = run_bass_kernel_spmd(_NC, maps, core_ids=list(range(8)))
    return np.stack([np.asarray(r["out"], dtype=np.float32) for r in res.results], axis=0)
```

```python
import math
import numpy as np
from contextlib import ExitStack
import concourse.bass as bass
import concourse.mybir as mybir
from concourse.bass_utils import run_bass_kernel_spmd

F32 = mybir.dt.float32
BF16 = mybir.dt.bfloat16
I32 = mybir.dt.int32
U32 = mybir.dt.uint32
AF = mybir.ActivationFunctionType
ALU = mybir.AluOpType
AX = mybir.AxisListType

S = 4096
C = 256
NTOK = S + C
D = 1024
KD = 8
INW = 1792
NE = 32
CAP = 1536
NSLOT = NE * CAP
EPS = 1e-6
BIG = 4.0e6
CSIG = float(np.float32(1.0 / (1.0 + math.exp(-1.702 * 7.0))))


class Buf:
    __slots__ = ("name", "w", "r")

    def __init__(self, name=""):
        self.name = name
        self.w = {}
        self.r = {}


class Eng:
    def __init__(self, name, h, sem):
        self.name, self.h, self.sem = name, h, sem
        self.count = 0
        self.waited = {}


class FW:
    def __init__(self, nc, stack):
        self.nc = nc
        self.stack = stack
        self.nsem = 0
        self.pe = self._eng("pe", nc.tensor)
        self.act = self._eng("act", nc.scalar)
        self.dve = self._eng("dve", nc.vector)
        self.pool = self._eng("pool", nc.gpsimd)
        self.sp = self._eng("sp", nc.sync)
        self.engs = [self.pe, self.act, self.dve, self.pool, self.sp]
        self.dma_sems = {}
        self.all_dma = []

    def new_sem(self, name):
        self.nsem += 1
        return self.stack.enter_context(self.nc.semaphore(name))

    def _eng(self, name, h):
        return Eng(name, h, self.new_sem("s_" + name))

    def _wait(self, eng, sem, val):
        key = sem.num
        if eng.waited.get(key, 0) >= val:
            return
        eng.waited[key] = val
        eng.h.wait_ge(sem, val)

    def _deps(self, eng, reads, writes, waw=True):
        for b in reads:
            for s, v in b.w.values():
                if s is eng.sem and eng is self.pe:
                    continue
                self._wait(eng, s, v)
        for b in writes:
            for d in ((b.w, b.r) if waw else (b.r,)):
                for s, v in d.values():
                    if s is eng.sem and eng is self.pe:
                        continue
                    self._wait(eng, s, v)

    @staticmethod
    def _rec(d, sem, val):
        k = sem.num
        if k not in d or d[k][1] < val:
            d[k] = (sem, val)

    def _mark(self, sem, val, reads, writes):
        for b in writes:
            b.r = {}
            self._rec(b.w, sem, val)
        for b in reads:
            self._rec(b.r, sem, val)

    def op(self, eng, fn, reads=(), writes=(), inc=True):
        self._deps(eng, reads, writes)
        ins = fn()
        if inc:
            eng.count += 1
            ins.then_inc(eng.sem, 1)
            self._mark(eng.sem, eng.count, reads, writes)
        else:
            self._mark(eng.sem, eng.count + 1, reads, writes)
        return ins

    def dma(self, q, fn, reads=(), writes=(), owner=None, waw=True):
        self._deps(q, reads, writes, waw=waw)
        owner = owner or (writes[0] if writes else reads[0])
        ent = self.dma_sems.get(id(owner))
        if ent is None:
            ent = [self.new_sem("d%d" % self.nsem), 0, owner]
            self.dma_sems[id(owner)] = ent
            self.all_dma.append(ent)
        ent[1] += 16
        ins = fn()
        ins.then_inc(ent[0], 16)
        for b in writes:
            if waw:
                b.r = {}
            self._rec(b.w, ent[0], ent[1])
        for b in reads:
            self._rec(b.r, ent[0], ent[1])
        return ins

    def share(self, owner, *others):
        ent = self.dma_sems.get(id(owner))
        if ent is None:
            ent = [self.new_sem("d%d" % self.nsem), 0, owner]
            self.dma_sems[id(owner)] = ent
            self.all_dma.append(ent)
        for o in others:
            self.dma_sems[id(o)] = ent

    def barrier(self):
        for e in self.engs:
            for x in self.engs:
                if x is not e and x.count > 0:
                    self._wait(e, x.sem, x.count)
            for ent in self.all_dma:
                if ent[1] > 0:
                    self._wait(e, ent[0], ent[1])


def build(nlayers=2, dbg=False, stop_after=None):
    nc = bass.Bass("TRN2", target_bir_lowering=False)

    def din(name, shape, dt=F32):
        return nc.dram_tensor(name, list(shape), dt, kind="ExternalInput").ap()

    def dscr(name, shape, dt, big=False):
        return nc.dram_tensor(name, list(shape), dt, kind="ExternalOutput" if (dbg and not big) else "Internal").ap()

    x_in = din("x", [S, D]); ctx_in = din("ctx", [C, D])
    cc_in = din("cc", [128, KD, 2])
    wada_in = din("w_ada", [2, D, 6 * D]); badac_in = din("b_adac", [128, 2, 48]); bada_in = din("b_ada", [2, 6 * D])
    g1c_in = din("g1c", [128, 2, KD]); g2bc_in = din("g2bc", [2, 128, D]); gfbc_in = din("gfbc", [128, D])
    win_in = din("w_in", [2, D, INW]); wo_in = din("w_o", [2, D, D]); wr_in = din("w_router", [2, D, NE])
    brbc_in = din("b_rbc", [128, 2, NE]); gqk_in = din("gqk", [128, 2, 640]); cs_in = din("cs", [128, 32, 64])
    wdwc_in = din("wdwc", [128, 2, 2, 31]); cvp_in = din("cvp", [128, 2, 2, 3])
    sgp_in = din("sgp", [128, 2, 2, 256]); wsT_in = din("wsT", [128, 2, 4, 128]); bsc_in = din("bsc", [128, 2, 4])
    wg_in = din("w_gate", [2, NE, D, D]); wu_in = din("w_up", [2, NE, D, D]); wd_in = din("w_down", [2, NE, D, D])
    bgu_in = din("bgu", [128, 2, NE, 2, KD]); bd_in = din("b_down", [2, NE, D])
    ident_in = din("ident", [128, 128]); tri_in = din("tri", [128, 128]); eoff_in = din("eoff", [128, NE])
    iota_in = din("iota", [128, NE])
    out_d = nc.dram_tensor("out", [S, D], F32, kind="ExternalOutput").ap()

    QS = dscr("QS", [NTOK, 512], BF16); KS = dscr("KS", [NTOK, 128], BF16)
    US = dscr("US", [16 + S + 32, 256], BF16); USC = dscr("USC", [16 + C + 32, 256], BF16)
    CS = dscr("CS", [NTOK, 256], BF16)
    XM = dscr("XM", [NTOK, D], F32); XS = dscr("XS", [NTOK, D], F32)
    XG = dscr("XG", [NSLOT, D], BF16, big=True); YG = dscr("YG", [NSLOT, D], F32, big=True)
    bQS, bKS, bUS, bUSC, bCS, bXM, bXS, bXG, bYG, bOUT = [Buf(n) for n in
                                                          "QS KS US USC CS XM XS XG YG OUT".split()]
    bIN = Buf("inputs")

    with ExitStack() as top:
        fw = FW(nc, top)
        uid = [0]

        def T(stack, shape, dt, name=None):
            uid[0] += 1
            return stack.enter_context(nc.sbuf_tensor(name or "t%d" % uid[0], list(shape), dt))

        def mk(eng, h):
            def f(name, R=(), W=(), inc=True, **kw):
                return fw.op(eng, lambda: getattr(h, name)(**kw), R, W, inc)
            return f
        dve = mk(fw.dve, nc.vector); act = mk(fw.act, nc.scalar); pool = mk(fw.pool, nc.gpsimd)

        def mm(out, lhsT, rhs, R, W, start=True, stop=True, inc=None):
            return fw.op(fw.pe, lambda: nc.tensor.matmul(out, lhsT=lhsT, rhs=rhs, start=start, stop=stop),
                         R, W, inc=(stop if inc is None else inc))

        def dma(out, in_, R, W, q="sp", waw=True, owner=None):
            h = nc.sync if q == "sp" else nc.gpsimd
            e = fw.sp if q == "sp" else fw.pool
            return fw.dma(e, lambda: h.dma_start(out=out, in_=in_), R, W, owner=owner, waw=waw)

        def dmaT(out, in_, R, W, waw=True, owner=None):
            return fw.dma(fw.sp, lambda: nc.sync.dma_start_transpose(out=out, in_=in_), R, W, owner=owner, waw=waw)

        def rsqrt_col(stack_tiles, src, R, dst, scale, n=1):
            tmp, btmp = stack_tiles
            act("activation", R=R, W=[btmp], out=tmp[:, 0:n], in_=src, func=AF.Sqrt, bias=EPS, scale=scale)
            return tmp, btmp

        bcreg = nc.gpsimd.alloc_register("bcreg")
        nc.gpsimd.reg_mov(bcreg, NSLOT - 1)
        ps = top.enter_context(nc.psum_tensor("ps", [128, 8, 512], F32))
        pb = [Buf("pb%d" % i) for i in range(8)]
        psf = ps[:].rearrange("p b n -> p (b n)")

        bP = Buf("params")
        ident_f = T(top, [128, 128], F32); tri_f = T(top, [128, 128], F32)
        ident_b = T(top, [128, 128], BF16); tri_b = T(top, [128, 128], BF16)
        ones_b = T(top, [128, 128], BF16); ones_f = T(top, [128, 128], F32); onesq = T(top, [128, 128], F32)
        eoff = T(top, [128, NE], F32); iota = T(top, [128, NE], F32)
        cc = T(top, [128, KD, 2], F32); scc = T(top, [128, KD, 2], F32)
        badac = T(top, [128, 2, 48], F32); g1c = T(top, [128, 2, KD], F32)
        brbc = T(top, [128, 2, NE], F32)
        wdwc = T(top, [128, 2, 2, 31], F32); cvp = T(top, [128, 2, 2, 3], F32)
        bsc = T(top, [128, 2, 4], F32)
        wr = T(top, [128, 2, KD, NE], F32)
        zt = T(top, [128, 256], BF16)
        for t_, s_ in ((ident_f, ident_in), (tri_f, tri_in), (eoff, eoff_in), (iota, iota_in), (cc, cc_in),
                       (badac, badac_in), (g1c, g1c_in), (brbc, brbc_in),
                       (wdwc, wdwc_in), (cvp, cvp_in), (bsc, bsc_in)):
            dma(t_[:], s_, R=[bIN], W=[bP], waw=False)
        for l in range(2):
            dma(wr[:, l], wr_in[l].rearrange("(k p) n -> p k n", p=128), R=[bIN], W=[bP], waw=False)
        bC = Buf("consts")
        dve("tensor_copy", R=[bP], W=[bC], out=ident_b[:], in_=ident_f[:])
        dve("tensor_copy", R=[bP], W=[bC], out=tri_b[:], in_=tri_f[:])
        dve("memset", W=[bC], ap=ones_b[:], constant=1.0)
        dve("memset", W=[bC], ap=ones_f[:], constant=1.0)
        dve("memset", W=[bC], ap=onesq[:], constant=1.0 / 256.0)
        dve("memset", W=[bC], ap=zt[:], constant=0.0)
        act("activation", R=[bP], W=[bC], out=scc[:], in_=cc[:], func=AF.Silu)
        dma(US[0:16, :], zt[0:16, :], R=[bC], W=[bUS], waw=False)
        dma(US[16 + S:16 + S + 32, :], zt[0:32, :], R=[bC], W=[bUS], waw=False)
        dma(USC[0:16, :], zt[0:16, :], R=[bC], W=[bUSC], waw=False)
        dma(USC[16 + C:16 + C + 32, :], zt[0:32, :], R=[bC], W=[bUSC], waw=False)

        A1 = T(top, [128, 2, KD], F32); B1 = T(top, [128, 2, KD], F32)
        BC = T(top, [128, 2, 4, D], F32)
        bMOD = Buf("mod")
        sm = T(top, [128, 64], F32); bsm = Buf("sm")
        junk = T(top, [128, D], BF16); bjunk = Buf("junk")

        def compute_mod(l):
            with ExitStack() as ph:
                slab = [T(ph, [128, KD, 512], F32) for _ in range(2)]
                bslab = [Buf("slab%d" % i) for i in range(2)]
                brow = [T(ph, [1, 512], F32) for _ in range(2)]
                bbrow = [Buf("brow%d" % i) for i in range(2)]
                modc = T(ph, [128, 16, 2], F32); bmodc = Buf("modc")
                g2t = T(ph, [128, D], F32); bg2t = Buf("g2t")
                scb = T(ph, [128, KD, 2, 128], F32); bscb = Buf("scb")
                dve("tensor_copy", R=[bC], W=[bscb], out=scb[:], in_=scc[:].unsqueeze(3).to_broadcast([128, KD, 2, 128]))
                dma(g2t[:], g2bc_in[l], R=[bIN], W=[bg2t])
                nwhich = 2 if l == 0 else 1
                for sidx in range(12):
                    sl, bsl = slab[sidx % 2], bslab[sidx % 2]
                    dma(sl[:], wada_in[l, :, sidx * 512:(sidx + 1) * 512].rearrange("(k p) n -> p k n", p=128),
                        R=[bIN], W=[bsl])
                    if sidx < 4:
                        for jj in range(4):
                            j = sidx * 4 + jj
                            for k in range(KD):
                                mm(ps[:, 7, j * 2:j * 2 + 2], sl[:, k, jj * 128:(jj + 1) * 128], scc[:, k, :],
                                   R=[bsl, bC], W=[pb[7]], start=(k == 0), stop=(k == KD - 1))
                        if sidx == 3:
                            dve("tensor_tensor", R=[pb[7], bP], W=[bmodc], out=modc[:],
                                in0=ps[:, 7, 0:32].rearrange("p (j w) -> p j w", w=2),
                                in1=badac[:, l, 0:16].unsqueeze(2).to_broadcast([128, 16, 2]), op=ALU.add)
                            for w in range(2):
                                dve("scalar_tensor_tensor", R=[bmodc, bP], W=[bMOD], out=A1[:, w, :],
                                    in0=modc[:, 8:16, w], scalar=1.0, in1=g1c[:, l, :], op0=ALU.add, op1=ALU.mult)
                                dve("tensor_copy", R=[bmodc], W=[bMOD], out=B1[:, w, :], in_=modc[:, 0:8, w])
                    else:
                        v = (sidx - 4) // 2
                        half = (sidx - 4) % 2
                        br, bbr = brow[sidx % 2], bbrow[sidx % 2]
                        dma(br[:], bada_in[l:l + 1, sidx * 512:(sidx + 1) * 512], R=[bIN], W=[bbr])
                        for w in range(nwhich):
                            bank = 5 + w
                            for k in range(KD):
                                mm(ps[:, bank, :], scb[:, k, w, :], sl[:, k, :], R=[bsl, bscb], W=[pb[bank]],
                                   start=(k == 0), stop=False)
                            mm(ps[:, bank, :], ones_f[0:1, :], br[0:1, :], R=[bbr, bC], W=[pb[bank]],
                               start=False, stop=True)
                            dst = BC[:, w, v, half * 512:(half + 1) * 512]
                            if v == 2:
                                dve("scalar_tensor_tensor", R=[pb[bank], bg2t], W=[bMOD], out=dst,
                                    in0=ps[:, bank, :], scalar=1.0, in1=g2t[:, half * 512:(half + 1) * 512],
                                    op0=ALU.add, op1=ALU.mult)
                            else:
                                act("copy", R=[pb[bank]], W=[bMOD], out=dst, in_=ps[:, bank, :])
                fw.barrier()

        def phase1(l, ph, KT, VA, bKT, bVA):
            WIN = T(ph, [128, KD, INW], BF16); bWIN = Buf("WIN")
            gqk = T(ph, [128, 640], F32); cs = T(ph, [128, 32, 64], F32)
            sgp = T(ph, [128, 2, 256], F32); wsT = T(ph, [128, 4, 128], BF16)
            bP1 = Buf("p1params")
            dma(gqk[:], gqk_in[:, l, :], R=[bIN], W=[bP1], waw=False)
            dma(cs[:], cs_in, R=[bIN], W=[bP1], waw=False)
            dma(sgp[:], sgp_in[:, l], R=[bIN], W=[bP1], waw=False)
            dma(wsT[:], wsT_in[:, l], R=[bIN], W=[bP1], q="pool", waw=False)
            dma(WIN[:], win_in[l].rearrange("(k p) n -> p k n", p=128), R=[bIN], W=[bWIN], q="pool")
            xt = [T(ph, [128, D], F32) for _ in range(2)]; bxt = [Buf("xt%d" % i) for i in range(2)]
            xn = [T(ph, [128, D], BF16) for _ in range(2)]; bxn = [Buf("xn%d" % i) for i in range(2)]
            hT = [T(ph, [128, KD, 128], BF16) for _ in range(2)]; bhT = [Buf("hT%d" % i) for i in range(2)]
            sq = T(ph, [128, 640], F32); bsq = Buf("sq")
            qn = T(ph, [128, 640], F32); bqn = Buf("qn")
            rt = T(ph, [128, 4, 320], F32); brt = Buf("rt")
            qkb = [T(ph, [128, 640], BF16) for _ in range(2)]; bqkb = [Buf("qkb%d" % i) for i in range(2)]
            sig = T(ph, [128, 256], F32); bsig = Buf("sig")
            ub = [T(ph, [128, 256], BF16) for _ in range(2)]; bub = [Buf("ub%d" % i) for i in range(2)]
            zg = T(ph, [128, 512], F32); bzg = Buf("zg")
            vn = T(ph, [128, 256], F32); bvn = Buf("vn")
            vln = T(ph, [128, 256], BF16); bvln = Buf("vln")
            cob = [T(ph, [128, 256], BF16) for _ in range(2)]; bcob = [Buf("cob%d" % i) for i in range(2)]
            st6 = T(ph, [128, 8], F32); bst6 = Buf("st6")
            c1 = T(ph, [128, 32], F32); bc1 = Buf("c1")
            dve("memset", W=[bVA], ap=VA[:, :, :, 64:128], constant=1.0)

            def load(t):
                if l == 0:
                    src = x_in[t * 128:(t + 1) * 128, :] if t < 32 else ctx_in[(t - 32) * 128:(t - 31) * 128, :]
                    dma(xt[t % 2][:], src, R=[bIN], W=[bxt[t % 2]])
                else:
                    dma(xt[t % 2][:], XS[t * 128:(t + 1) * 128, :], R=[bXS], W=[bxt[t % 2]])
            load(0)
            for t in range(34):
                if t + 1 < 34:
                    load(t + 1)
                w = 0 if t < 32 else 1
                X, bX = xt[t % 2], bxt[t % 2]
                XN, bXN = xn[t % 2], bxn[t % 2]
                H, bH = hT[t % 2], bhT[t % 2]
                QB, bQB = qkb[t % 2], bqkb[t % 2]
                act("activation", R=[bX], W=[bjunk, bc1], out=junk[:], in_=X[:], func=AF.Square, accum_out=c1[:, 0:1])
                act("activation", R=[bc1], W=[bc1], out=c1[:, 1:2], in_=c1[:, 0:1], func=AF.Sqrt, bias=EPS, scale=1.0 / D)
                dve("reciprocal", R=[bc1], W=[bc1], out=c1[:, 2:3], in_=c1[:, 1:2])
                dve("tensor_scalar", R=[bX, bc1], W=[bXN], out=XN[:], in0=X[:], scalar1=c1[:, 2:3], scalar2=None, op0=ALU.mult)
                for k in range(KD):
                    mm(ps[:, k // 4, (k % 4) * 128:(k % 4 + 1) * 128], XN[:, k * 128:(k + 1) * 128], ident_b[:],
                       R=[bXN, bC], W=[pb[k // 4]], inc=(k % 4 == 3))
                for k in range(KD):
                    src = ps[:, k // 4, (k % 4) * 128:(k % 4 + 1) * 128]
                    if k % 2 == 0:
                        act("activation", R=[pb[k // 4], bMOD], W=[bH], out=H[:, k, :], in_=src, func=AF.Identity,
                            scale=A1[:, w, k:k + 1], bias=B1[:, w, k:k + 1])
                    else:
                        dve("tensor_scalar", R=[pb[k // 4], bMOD], W=[bH], out=H[:, k, :], in0=src,
                            scalar1=A1[:, w, k:k + 1], scalar2=B1[:, w, k:k + 1], op0=ALU.mult, op1=ALU.add)
                for n in range(4):
                    n0 = n * 512
                    wd_ = min(512, INW - n0)
                    for k in range(KD):
                        mm(ps[:, 2 + n, 0:wd_], H[:, k, :], WIN[:, k, n0:n0 + wd_], R=[bH, bWIN], W=[pb[2 + n]],
                           start=(k == 0), stop=(k == KD - 1))
                P0, P1, P2, P3 = ps[:, 2, :], ps[:, 3, :], ps[:, 4, :], ps[:, 5, :]
                act("activation", R=[pb[2]], W=[bsq], out=sq[:, 0:512], in_=P0, func=AF.Square)
                act("activation", R=[pb[3]], W=[bsq], out=sq[:, 512:640], in_=P1[:, 0:128], func=AF.Square)
                dve("tensor_reduce", R=[bsq], W=[bc1], out=c1[:, 4:14], in_=sq[:].rearrange("p (h d) -> p h d", d=64),
                    axis=AX.X, op=ALU.add)
                act("activation", R=[bc1], W=[bc1], out=c1[:, 14:24], in_=c1[:, 4:14], func=AF.Sqrt, bias=EPS, scale=1.0 / 64)
                dve("reciprocal", R=[bc1], W=[bc1], out=c1[:, 4:14], in_=c1[:, 14:24])
                dve("tensor_tensor", R=[pb[2], bc1], W=[bqn], out=qn[:, 0:512].rearrange("p (h d) -> p h d", d=64),
                    in0=P0.rearrange("p (h d) -> p h d", d=64),
                    in1=c1[:, 4:12].unsqueeze(2).to_broadcast([128, 8, 64]), op=ALU.mult)
                dve("tensor_tensor", R=[pb[3], bc1], W=[bqn], out=qn[:, 512:640].rearrange("p (h d) -> p h d", d=64),
                    in0=P1[:, 0:128].rearrange("p (h d) -> p h d", d=64),
                    in1=c1[:, 12:14].unsqueeze(2).to_broadcast([128, 2, 64]), op=ALU.mult)
                if t < 32:
                    pool("tensor_tensor", R=[bqn, bP1], W=[bqn], out=qn[:], in0=qn[:], in1=gqk[:], op=ALU.mult)
                    q3 = qn[:].rearrange("p (h d) -> p h d", d=64)
                    o3 = QB[:].rearrange("p (h d) -> p h d", d=64)
                    cosb = cs[:, t, 0:32].unsqueeze(1).to_broadcast([128, 10, 32])
                    sinb = cs[:, t, 32:64].unsqueeze(1).to_broadcast([128, 10, 32])
                    r3 = rt[:].rearrange("p a (h d) -> p a h d", d=32)
                    dve("tensor_tensor", R=[bqn, bP1], W=[brt], out=r3[:, 0], in0=q3[:, :, 0:32], in1=cosb, op=ALU.mult)
                    pool("tensor_tensor", R=[bqn, bP1], W=[brt], out=r3[:, 1], in0=q3[:, :, 32:64], in1=sinb, op=ALU.mult)
                    dve("tensor_tensor", R=[bqn, bP1], W=[brt], out=r3[:, 2], in0=q3[:, :, 32:64], in1=cosb, op=ALU.mult)
                    pool("tensor_tensor", R=[bqn, bP1], W=[brt], out=r3[:, 3], in0=q3[:, :, 0:32], in1=sinb, op=ALU.mult)
                    dve("tensor_tensor", R=[brt], W=[bQB], out=o3[:, :, 0:32], in0=r3[:, 0], in1=r3[:, 1], op=ALU.subtract)
                    pool("tensor_tensor", R=[brt], W=[bQB], out=o3[:, :, 32:64], in0=r3[:, 2], in1=r3[:, 3], op=ALU.add)
                else:
                    pool("tensor_tensor", R=[bqn, bP1], W=[bQB], out=QB[:], in0=qn[:], in1=gqk[:], op=ALU.mult)
                for j in range(4):
                    dma(QS[t * 128:(t + 1) * 128, j * 128:(j + 1) * 128].rearrange("p (g d) -> p g d", g=2),
                        QB[:, 0:512].rearrange("p (g j d) -> p j g d", g=2, j=4)[:, j], R=[bQB], W=[bQS], waw=False)
                dma(KS[t * 128:(t + 1) * 128, :], QB[:, 512:640], R=[bQB], W=[bKS], waw=False)
                act("copy", R=[pb[3]], W=[bVA], out=VA[:, t, :, 0:64], in_=P1[:, 128:256].rearrange("p (g d) -> p g d", d=64))
                UB, bUB = ub[t % 2], bub[t % 2]
                act("activation", R=[pb[4]], W=[bsig], out=sig[:], in_=P2[:, 0:256], func=AF.Sigmoid)
                dve("tensor_tensor", R=[pb[3], bsig], W=[bUB], out=UB[:], in0=P1[:, 256:512], in1=sig[:], op=ALU.mult)
                if t < 32:
                    dma(US[16 + t * 128:16 + (t + 1) * 128, :], UB[:], R=[bUB], W=[bUS], waw=False)
                else:
                    dma(USC[16 + (t - 32) * 128:16 + (t - 31) * 128, :], UB[:], R=[bUB], W=[bUSC], waw=False)
                act("activation", R=[pb[4], pb[5]], W=[bzg], out=zg[:], in_=psf[:, 4 * 512 + 256:5 * 512 + 256], func=AF.Gelu)
                dve("bn_stats", R=[bzg], W=[bst6], out=st6[:, 0:6], in_=zg[:, 256:512])
                dve("bn_aggr", R=[bst6], W=[bst6], out=st6[:, 6:8], in_=st6[:, 0:6])
                act("activation", R=[bst6], W=[bc1], out=c1[:, 24:25], in_=st6[:, 7:8], func=AF.Sqrt, bias=EPS, scale=1.0)
                dve("reciprocal", R=[bc1], W=[bc1], out=c1[:, 25:26], in_=c1[:, 24:25])
                dve("tensor_scalar", R=[bzg, bst6, bc1], W=[bvn], out=vn[:], in0=zg[:, 256:512], scalar1=st6[:, 6:7],
                    scalar2=c1[:, 25:26], op0=ALU.subtract, op1=ALU.mult)
                pool("tensor_tensor", R=[bvn, bP1], W=[bvn], out=vn[:], in0=vn[:], in1=sgp[:, 0, :], op=ALU.mult)
                pool("tensor_tensor", R=[bvn, bP1], W=[bvln], out=vln[:], in0=vn[:], in1=sgp[:, 1, :], op=ALU.add)
                for h in range(4):
                    mm(ps[:, 6, h * 64:(h + 1) * 64], wsT[:, h, :], vln[:, h * 64:(h + 1) * 64], R=[bvln, bP1], W=[pb[6]],
                       inc=(h == 3))
                CO, bCO = cob[t % 2], bcob[t % 2]
                for h in range(4):
                    dve("scalar_tensor_tensor", R=[pb[6], bzg, bP], W=[bCO], out=CO[:, h * 64:(h + 1) * 64],
                        in0=ps[:, 6, h * 64:(h + 1) * 64], scalar=bsc[:, l, h:h + 1], in1=zg[:, h * 64:(h + 1) * 64],
                        op0=ALU.add, op1=ALU.mult)
                dma(CS[t * 128:(t + 1) * 128, :], CO[:], R=[bCO], W=[bCS], waw=False)
            for i in range(34):
                dmaT(KT[:, i * 128:(i + 1) * 128], KS[i * 128:(i + 1) * 128, :], R=[bKS], W=[bKT], waw=False)

        def phase2(l, ph, KT, VA, bKT, bVA, DEST, W4, bRT):
            WO = T(ph, [128, KD, D], BF16); bWO = Buf("WO")
            dma(WO[:], wo_in[l].rearrange("(k p) n -> p k n", p=128), R=[bIN], W=[bWO], q="pool")
            DG = T(ph, [128, 2, 31, 128], BF16); bDG = Buf("DG")
            for c_ in range(2):
                for k in range(31):
                    e_ = dve if (k % 2 == 0) else pool
                    e_("tensor_scalar", R=[bC, bP], W=[bDG], out=DG[:, c_, k, :], in0=ident_f[:],
                       scalar1=wdwc[:, l, c_, k:k + 1], scalar2=None, op0=ALU.mult)
            QTb = [T(ph, [128, 4, 512], BF16) for _ in range(2)]; bQTb = [Buf("QTb%d" % i) for i in range(2)]
            UTb = [T(ph, [128, 2, 544], BF16) for _ in range(2)]; bUTb = [Buf("UTb%d" % i) for i in range(2)]
            CTb = [T(ph, [128, 2, 512], BF16) for _ in range(2)]; bCTb = [Buf("CTb%d" % i) for i in range(2)]
            cv = T(ph, [128, 2, 512], F32); bcv = Buf("cv")
            sqv = T(ph, [128, 2, 512], F32); bsqv = Buf("sqv")
            msq = T(ph, [128, 512], F32); bmsq = Buf("msq")
            rstd = T(ph, [128, 512], F32); brstd = Buf("rstd")
            CVb = T(ph, [128, 2, 512], BF16); bCVb = Buf("CVb")
            PT = [T(ph, [128, 2, 512], BF16) for _ in range(2)]; bPT = [Buf("PT%d" % i) for i in range(2)]
            AT = T(ph, [128, 4, 512], BF16); bAT = Buf("AT")
            rec = T(ph, [64, 512], F32); brec = Buf("rec")
            xr = [T(ph, [128, D], F32) for _ in range(2)]; bxr = [Buf("xr%d" % i) for i in range(2)]
            xm = [T(ph, [128, D], F32) for _ in range(2)]; bxm = [Buf("xm%d" % i) for i in range(2)]
            tmp = T(ph, [128, D], F32); btmp = Buf("tmp")
            h2 = T(ph, [128, D], F32); bh2 = Buf("h2")
            h2b = [T(ph, [128, D], BF16) for _ in range(2)]; bh2b = [Buf("h2b%d" % i) for i in range(2)]
            h2T = T(ph, [128, KD, 128], F32); bh2T = Buf("h2T")
            c1 = T(ph, [128, 8], F32); bc1 = Buf("c1b")
            lg = T(ph, [128, NE], F32); blg = Buf("lg")
            mx8 = T(ph, [128, 8], F32); bmx8 = Buf("mx8")
            ix8 = T(ph, [128, 8], U32); bix8 = Buf("ix8")
            ixf = T(ph, [128, 8], F32); bixf = Buf("ixf")
            e4 = T(ph, [128, 8], F32); be4 = Buf("e4")
            mask = T(ph, [128, NE], BF16); bmask = Buf("mask")
            cnt = T(ph, [128, NE], F32); bcnt = Buf("cnt")
            posf = T(ph, [128, NE], F32); bposf = Buf("posf")
            val = T(ph, [128, NE], F32); bval = Buf("val")
            oh = T(ph, [128, 4, NE], F32); boh = Buf("oh")
            dk = T(ph, [128, 8], F32); bdk = Buf("dk")
            dve("memset", W=[bcnt], ap=cnt[:], constant=0.0)

            blocks = [(b * 512, 512, list(range(34)), 0) for b in range(8)]
            if l == 0:
                blocks.append((S, 256, [32, 33], 1))

            def loads(bi):
                tok0, nt, kcs, w = blocks[bi]
                Q, bQ = QTb[bi % 2], bQTb[bi % 2]
                U, bU = UTb[bi % 2], bUTb[bi % 2]
                Cc, bCc = CTb[bi % 2], bCTb[bi % 2]
                for j in range(4):
                    dmaT(Q[:, j, 0:nt], QS[tok0:tok0 + nt, j * 128:(j + 1) * 128], R=[bQS], W=[bQ], waw=(j == 0))
                for c_ in range(2):
                    if w == 0:
                        dmaT(U[:, c_, 0:nt + 32], US[tok0:tok0 + nt + 32, c_ * 128:(c_ + 1) * 128], R=[bUS], W=[bU], waw=(c_ == 0))
                    else:
                        dmaT(U[:, c_, 0:nt + 32], USC[0:nt + 32, c_ * 128:(c_ + 1) * 128], R=[bUSC], W=[bU], waw=(c_ == 0))
                    dmaT(Cc[:, c_, 0:nt], CS[tok0:tok0 + nt, c_ * 128:(c_ + 1) * 128], R=[bCS], W=[bCc], waw=(c_ == 0))

            loads(0)
            for bi, (tok0, nt, kcs, w) in enumerate(blocks):
                if bi + 1 < len(blocks):
                    loads(bi + 1)
                Q, bQ = QTb[bi % 2], bQTb[bi % 2]
                U, bU = UTb[bi % 2], bUTb[bi % 2]
                Cc, bCc = CTb[bi % 2], bCTb[bi % 2]
                for c_ in range(2):
                    for k in range(31):
                        mm(ps[:, c_, 0:nt], DG[:, c_, k, :], U[:, c_, 1 + k:1 + k + nt], R=[bDG, bU], W=[pb[c_]],
                           start=(k == 0), stop=(k == 30))
                    act("activation", R=[pb[c_], bP], W=[bcv], out=cv[:, c_, 0:nt], in_=ps[:, c_, 0:nt], func=AF.Identity,
                        bias=cvp[:, l, c_, 0:1], scale=1.0)
                    act("activation", R=[bcv], W=[bsqv], out=sqv[:, c_, 0:nt], in_=cv[:, c_, 0:nt], func=AF.Square)
                for c_ in range(2):
                    mm(ps[:, 2, 0:nt], onesq[:], cv[:, c_, 0:nt], R=[bcv, bC], W=[pb[2]], start=(c_ == 0), stop=(c_ == 1))
                for c_ in range(2):
                    mm(ps[:, 0, 0:nt], onesq[:], sqv[:, c_, 0:nt], R=[bsqv, bC], W=[pb[0]], start=(c_ == 0), stop=(c_ == 1))
                act("activation", R=[pb[2]], W=[bmsq], out=msq[:, 0:nt], in_=ps[:, 2, 0:nt], func=AF.Square)
                dve("tensor_tensor", R=[pb[0], bmsq], W=[bmsq], out=msq[:, 0:nt], in0=ps[:, 0, 0:nt], in1=msq[:, 0:nt], op=ALU.subtract)
                act("activation", R=[bmsq], W=[brstd], out=rstd[:, 0:nt], in_=msq[:, 0:nt], func=AF.Sqrt, bias=EPS, scale=1.0)
                dve("reciprocal", R=[brstd], W=[brstd], out=rstd[:, 0:nt], in_=rstd[:, 0:nt])
                for c_ in range(2):
                    dve("tensor_tensor", R=[bcv, pb[2]], W=[bcv], out=cv[:, c_, 0:nt], in0=cv[:, c_, 0:nt], in1=ps[:, 2, 0:nt], op=ALU.subtract)
                    pool("tensor_tensor", R=[bcv, brstd], W=[bcv], out=cv[:, c_, 0:nt], in0=cv[:, c_, 0:nt], in1=rstd[:, 0:nt], op=ALU.mult)
                    act("activation", R=[bcv, bP], W=[bCVb], out=CVb[:, c_, 0:nt], in_=cv[:, c_, 0:nt], func=AF.Silu,
                        scale=cvp[:, l, c_, 1:2], bias=cvp[:, l, c_, 2:3])
                steps = [(h, i) for h in range(8) for i in range(len(kcs))]

                npairs = len(steps) // 2

                def score_pair(p):
                    for u_ in range(2):
                        h, i = steps[2 * p + u_]
                        g, j = h // 4, h % 4
                        kc = kcs[i]
                        bk = 2 * (p % 2) + u_
                        mm(ps[:, bk, 0:nt], KT[g * 64:(g + 1) * 64, kc * 128:(kc + 1) * 128], Q[g * 64:(g + 1) * 64, j, 0:nt],
                           R=[bKT, bQ], W=[pb[bk]])
                score_pair(0)
                if npairs > 1:
                    score_pair(1)
                for p in range(npairs):
                    q_ = p % 2
                    P2, bP2 = PT[q_], bPT[q_]
                    act("activation", R=[pb[2 * q_], pb[2 * q_ + 1]], W=[bP2], out=P2[:, :, 0:nt], in_=ps[:, 2 * q_:2 * q_ + 2, 0:nt],
                        func=AF.Exp, scale=0.125)
                    for u_ in range(2):
                        h, i = steps[2 * p + u_]
                        g = h // 4
                        kc = kcs[i]
                        ob = 4 + (h % 2)
                        mm(ps[:, ob, 0:nt], VA[:, kc, g, :], P2[:, u_, 0:nt], R=[bVA, bP2], W=[pb[ob]],
                           start=(i == 0), stop=(i == len(kcs) - 1), inc=True)
                    if p + 2 < npairs:
                        score_pair(p + 2)
                    h, i = steps[2 * p + 1]
                    if i == len(kcs) - 1:
                        ob = 4 + (h % 2)
                        dve("reciprocal", R=[pb[ob]], W=[brec], out=rec[:, 0:nt], in_=ps[64:128, ob, 0:nt])
                        dve("tensor_tensor", R=[pb[ob], brec], W=[bAT], out=AT[(h % 2) * 64:(h % 2) * 64 + 64, h // 2, 0:nt],
                            in0=ps[0:64, ob, 0:nt], in1=rec[:, 0:nt], op=ALU.mult)
                for s_ in range(nt // 128):
                    t = (tok0 // 128) + s_
                    cs_ = slice(s_ * 128, (s_ + 1) * 128)
                    XR, bXR = xr[t % 2], bxr[t % 2]
                    XMt, bXMt = xm[t % 2], bxm[t % 2]
                    H2B, bH2B = h2b[t % 2], bh2b[t % 2]
                    if l == 0:
                        src = x_in[t * 128:(t + 1) * 128, :] if t < 32 else ctx_in[(t - 32) * 128:(t - 31) * 128, :]
                        dma(XR[:], src, R=[bIN], W=[bXR])
                    else:
                        dma(XR[:], XS[t * 128:(t + 1) * 128, :], R=[bXS], W=[bXR])
                    for n in range(2):
                        for kk in range(KD):
                            if kk < 4:
                                lh, bl = AT[:, kk, cs_], bAT
                            elif kk < 6:
                                lh, bl = CVb[:, kk - 4, cs_], bCVb
                            else:
                                lh, bl = Cc[:, kk - 6, cs_], bCc
                            mm(ps[:, 6 + n, :], lh, WO[:, kk, n * 512:(n + 1) * 512], R=[bl, bWO], W=[pb[6 + n]],
                               start=(kk == 0), stop=(kk == KD - 1))
                    dve("tensor_tensor", R=[pb[6], pb[7], bMOD], W=[btmp], out=tmp[:], in0=psf[:, 6 * 512:8 * 512], in1=BC[:, w, 0, :], op=ALU.mult)
                    pool("tensor_tensor", R=[btmp, bXR], W=[bXMt], out=XMt[:], in0=tmp[:], in1=XR[:], op=ALU.add)
                    dma(XM[t * 128:(t + 1) * 128, :], XMt[:], R=[bXMt], W=[bXM], waw=False)
                    act("activation", R=[bXMt], W=[bjunk, bc1], out=junk[:], in_=XMt[:], func=AF.Square, accum_out=c1[:, 0:1])
                    act("activation", R=[bc1], W=[bc1], out=c1[:, 1:2], in_=c1[:, 0:1], func=AF.Sqrt, bias=EPS, scale=1.0 / D)
                    dve("reciprocal", R=[bc1], W=[bc1], out=c1[:, 2:3], in_=c1[:, 1:2])
                    dve("scalar_tensor_tensor", R=[bXMt, bc1, bMOD], W=[btmp], out=tmp[:], in0=XMt[:], scalar=c1[:, 2:3], in1=BC[:, w, 2, :],
                        op0=ALU.mult, op1=ALU.mult)
                    pool("tensor_tensor", R=[btmp, bMOD], W=[bh2], out=h2[:], in0=tmp[:], in1=BC[:, w, 1, :], op=ALU.add)
                    act("copy", R=[bh2], W=[bH2B], out=H2B[:], in_=h2[:])
                    for k in range(KD):
                        mm(ps[:, k // 4, (k % 4) * 128:(k % 4 + 1) * 128], h2[:, k * 128:(k + 1) * 128], ident_f[:],
                           R=[bh2, bP], W=[pb[k // 4]], inc=(k % 4 == 3))
                    act("copy", R=[pb[0]], W=[bh2T], out=h2T[:, 0:4, :], in_=ps[:, 0, :].rearrange("p (k n) -> p k n", n=128))
                    dve("tensor_copy", R=[pb[1]], W=[bh2T], out=h2T[:, 4:8, :], in_=ps[:, 1, :].rearrange("p (k n) -> p k n", n=128))
                    for k in range(KD):
                        mm(ps[:, 4, 0:NE], h2T[:, k, :], wr[:, l, k, :], R=[bh2T, bP], W=[pb[4]], start=(k == 0), stop=(k == KD - 1))
                    dve("tensor_tensor", R=[pb[4], bP], W=[blg], out=lg[:], in0=ps[:, 4, 0:NE], in1=brbc[:, l, :], op=ALU.add)
                    dve("max", R=[blg], W=[bmx8], out=mx8[:], in_=lg[:])
                    dve("max_index", R=[blg, bmx8], W=[bix8], out=ix8[:], in_max=mx8[:], in_values=lg[:])
                    dve("tensor_copy", R=[bix8], W=[bixf], out=ixf[:], in_=ix8[:])
                    dve("tensor_scalar", R=[bmx8], W=[be4], out=e4[:, 4:5], in0=mx8[:, 0:1], scalar1=-1.0, scalar2=None, op0=ALU.mult)
                    act("activation", R=[bmx8, be4], W=[be4], out=e4[:, 0:4], in_=mx8[:, 0:4], func=AF.Exp, bias=e4[:, 4:5], scale=1.0,
                        accum_out=e4[:, 5:6])
                    dve("reciprocal", R=[be4], W=[be4], out=e4[:, 6:7], in_=e4[:, 5:6])
                    dve("tensor_scalar", R=[blg, bmx8], W=[bmask], out=mask[:], in0=lg[:], scalar1=mx8[:, 3:4], scalar2=None, op0=ALU.is_ge)
                    mm(ps[:, 4, 32:64], tri_b[:], mask[:], R=[bmask, bC], W=[pb[4]])
                    mm(ps[:, 4, 64:96], ones_b[:], mask[:], R=[bmask, bC], W=[pb[4]])
                    dve("tensor_tensor", R=[pb[4], bcnt], W=[bposf], out=posf[:], in0=ps[:, 4, 32:64], in1=cnt[:], op=ALU.add)
                    dve("tensor_tensor", R=[pb[4], bcnt], W=[bcnt], out=cnt[:], in0=ps[:, 4, 64:96], in1=cnt[:], op=ALU.add)
                    dve("tensor_scalar", R=[bposf], W=[bval], out=val[:], in0=posf[:], scalar1=float(CAP), scalar2=None, op0=ALU.is_lt)
                    dve("tensor_tensor", R=[bposf, bP], W=[bposf], out=posf[:], in0=posf[:], in1=eoff[:], op=ALU.add)
                    dve("tensor_scalar", R=[bposf], W=[bposf], out=posf[:], in0=posf[:], scalar1=-BIG, scalar2=None, op0=ALU.add)
                    dve("tensor_tensor", R=[bposf, bval], W=[bposf], out=posf[:], in0=posf[:], in1=val[:], op=ALU.mult)
                    dve("tensor_scalar", R=[bposf], W=[bposf], out=posf[:], in0=posf[:], scalar1=BIG, scalar2=None, op0=ALU.add)
                    for k in range(4):
                        dve("tensor_scalar", R=[bixf, bP], W=[boh], out=oh[:, k, :], in0=iota[:], scalar1=ixf[:, k:k + 1], scalar2=None,
                            op0=ALU.is_equal)
                    dve("tensor_tensor", R=[boh, bposf], W=[boh], out=oh[:], in0=oh[:], in1=posf[:].unsqueeze(1).to_broadcast([128, 4, NE]),
                        op=ALU.mult)
                    dve("tensor_reduce", R=[boh], W=[bdk], out=dk[:, 0:4], in_=oh[:], axis=AX.X, op=ALU.add)
                    dve("tensor_copy", R=[bdk], W=[bRT], out=DEST[:, t, :], in_=dk[:, 0:4])
                    dve("tensor_scalar", R=[bdk], W=[bdk], out=dk[:, 4:8], in0=dk[:, 0:4], scalar1=float(NSLOT), scalar2=None, op0=ALU.is_lt)
                    dve("tensor_scalar", R=[be4], W=[be4], out=e4[:, 0:4], in0=e4[:, 0:4], scalar1=e4[:, 6:7], scalar2=None, op0=ALU.mult)
                    dve("tensor_tensor", R=[be4, bdk], W=[bRT], out=W4[:, t, :], in0=e4[:, 0:4], in1=dk[:, 4:8], op=ALU.mult)
                    for k in range(4):
                        fw.dma(fw.pool, lambda k=k: nc.gpsimd.indirect_dma_start(
                            out=XG, out_offset=bass.IndirectOffsetOnAxis(ap=DEST[:, t, k:k + 1], axis=0),
                            in_=H2B[:], in_offset=None, bounds_check=bcreg, oob_is_err=False),
                            reads=[bH2B, bRT], writes=[bXG], waw=False)

        def phase3(l, ph):
            NB = CAP // 512
            WB = [T(ph, [128, KD, D], BF16) for _ in range(4)]; bWB = [Buf("WB%d" % i) for i in range(4)]
            BD = [T(ph, [1, D], BF16) for _ in range(2)]; bBD = [Buf("BD%d" % i) for i in range(2)]
            bgu = T(ph, [128, NE, 2, KD], F32); bBGU = Buf("bgu")
            dma(bgu[:], bgu_in[:, l], R=[bIN], W=[bBGU])
            xT = [T(ph, [128, KD, 512], BF16) for _ in range(2)]; bxT = [Buf("xT%d" % i) for i in range(2)]
            aT = [T(ph, [128, KD, 512], BF16) for _ in range(2)]; baT = [Buf("aT%d" % i) for i in range(2)]
            gb = [T(ph, [128, 512], F32) for _ in range(2)]; bgb = [Buf("gb%d" % i) for i in range(2)]
            sg = [T(ph, [128, 512], F32) for _ in range(2)]; bsg = [Buf("sg%d" % i) for i in range(2)]
            ubt = [T(ph, [128, 512], F32) for _ in range(2)]; bubt = [Buf("ubt%d" % i) for i in range(2)]
            yt = [T(ph, [128, D], F32) for _ in range(2)]; byt = [Buf("yt%d" % i) for i in range(2)]
            wsrc = (wg_in, wu_in, wd_in)

            def mload(m):
                e, j = m // 3, m % 3
                dma(WB[m % 4][:], wsrc[j][l, e].rearrange("(k p) n -> p k n", p=128), R=[bIN], W=[bWB[m % 4]], q="pool")
                if j == 2:
                    dma(BD[e % 2][:], bd_in[l, e:e + 1, :], R=[bIN], W=[bBD[e % 2]], q="pool")

            items = [(e, b) for e in range(NE) for b in range(NB)]

            def xload(ii):
                e, b = items[ii]
                r0 = e * CAP + b * 512
                for k in range(KD):
                    dmaT(xT[ii % 2][:, k, :], XG[r0:r0 + 512, k * 128:(k + 1) * 128], R=[bXG], W=[bxT[ii % 2]], waw=(k == 0))
            nload = [0]

            def prefetch(upto):
                while nload[0] <= upto and nload[0] < 3 * NE:
                    mload(nload[0])
                    nload[0] += 1
            prefetch(2)
            xload(0)
            fcount = 0
            ycount = 0
            for ii, (e, b) in enumerate(items):
                WGe, bWGe = WB[(3 * e) % 4], bWB[(3 * e) % 4]
                WUe, bWUe = WB[(3 * e + 1) % 4], bWB[(3 * e + 1) % 4]
                WDe, bWDe = WB[(3 * e + 2) % 4], bWB[(3 * e + 2) % 4]
                if b == 0:
                    prefetch(3 * e + 3)
                if ii + 1 < len(items):
                    xload(ii + 1)
                X, bX = xT[ii % 2], bxT[ii % 2]
                A, bA = aT[ii % 2], baT[ii % 2]
                for f in range(KD):
                    pg = (fcount % 2) * 2
                    fcount += 1
                    for k in range(KD):
                        mm(ps[:, pg, :], WGe[:, k, f * 128:(f + 1) * 128], X[:, k, :], R=[bWGe, bX], W=[pb[pg]],
                           start=(k == 0), stop=(k == KD - 1))
                    for k in range(KD):
                        mm(ps[:, pg + 1, :], WUe[:, k, f * 128:(f + 1) * 128], X[:, k, :], R=[bWUe, bX], W=[pb[pg + 1]],
                           start=(k == 0), stop=(k == KD - 1))
                    G, bG = gb[f % 2], bgb[f % 2]
                    SG, bSG = sg[f % 2], bsg[f % 2]
                    UU, bUU = ubt[f % 2], bubt[f % 2]
                    dve("tensor_scalar", R=[pb[pg], bBGU], W=[bG], out=G[:], in0=ps[:, pg, :], scalar1=bgu[:, e, 0, f:f + 1], scalar2=7.0,
                        op0=ALU.add, op1=ALU.min)
                    act("activation", R=[bG], W=[bSG], out=SG[:], in_=G[:], func=AF.Sigmoid, scale=1.702)
                    dve("tensor_scalar", R=[pb[pg + 1], bBGU], W=[bUU], out=UU[:], in0=ps[:, pg + 1, :], scalar1=bgu[:, e, 1, f:f + 1],
                        scalar2=7.0, op0=ALU.add, op1=ALU.min)
                    dve("tensor_scalar", R=[bUU], W=[bUU], out=UU[:], in0=UU[:], scalar1=-7.0, scalar2=1.0, op0=ALU.max, op1=ALU.add)
                    dve("tensor_tensor", R=[bG, bSG], W=[bSG], out=SG[:], in0=G[:], in1=SG[:], op=ALU.mult)
                    dve("tensor_tensor", R=[bUU, bSG], W=[bA], out=A[:, f, :], in0=UU[:], in1=SG[:], op=ALU.mult)
                for s4 in range(4):
                    Y, bY = yt[ycount % 2], byt[ycount % 2]
                    pbase = 4 + (ycount % 2) * 2
                    ycount += 1
                    for n in range(2):
                        for f in range(KD):
                            mm(ps[:, pbase + n, :], A[:, f, s4 * 128:(s4 + 1) * 128], WDe[:, f, n * 512:(n + 1) * 512],
                               R=[bA, bWDe], W=[pb[pbase + n]], start=(f == 0), stop=False)
                        mm(ps[:, pbase + n, :], ones_b[0:1, :], BD[e % 2][0:1, n * 512:(n + 1) * 512], R=[bBD[e % 2], bC], W=[pb[pbase + n]],
                           start=False, stop=True)
                    act("copy", R=[pb[pbase]], W=[bY], out=Y[:, 0:512], in_=ps[:, pbase, :])
                    dve("tensor_copy", R=[pb[pbase + 1]], W=[bY], out=Y[:, 512:1024], in_=ps[:, pbase + 1, :])
                    r0 = e * CAP + b * 512 + s4 * 128
                    dma(YG[r0:r0 + 128, :], Y[:], R=[bY], W=[bYG], waw=False)

        def phase4(l, ph, DEST, W4, bRT):
            last = (l == nlayers - 1)
            Y4 = [T(ph, [128, 4, D], F32) for _ in range(2)]; bY4 = [Buf("Y4%d" % i) for i in range(2)]
            xmt = [T(ph, [128, D], F32) for _ in range(2)]; bxmt = [Buf("xmt%d" % i) for i in range(2)]
            acc = T(ph, [128, D], F32); bacc = Buf("acc")
            xo = [T(ph, [128, D], F32) for _ in range(2)]; bxo = [Buf("xo%d" % i) for i in range(2)]
            c1 = T(ph, [128, 8], F32); bc1 = Buf("c1c")
            gfbc = T(ph, [128, D], F32); bGF = Buf("gfbc")
            if last:
                dma(gfbc[:], gfbc_in, R=[bIN], W=[bGF])
            for i in range(2):
                pool("memset", W=[bY4[i]], ap=Y4[i][:], constant=0.0)
            ntile = 32 if last else 34

            def loads(t):
                dma(xmt[t % 2][:], XM[t * 128:(t + 1) * 128, :], R=[bXM], W=[bxmt[t % 2]])
                for k in range(4):
                    fw.dma(fw.pool, lambda k=k: nc.gpsimd.indirect_dma_start(
                        out=Y4[t % 2][:, k, :], out_offset=None, in_=YG,
                        in_offset=bass.IndirectOffsetOnAxis(ap=DEST[:, t, k:k + 1], axis=0),
                        bounds_check=bcreg, oob_is_err=False),
                        reads=[bYG, bRT], writes=[bY4[t % 2]], waw=(k == 0))
            loads(0)
            for t in range(ntile):
                if t + 1 < ntile:
                    loads(t + 1)
                w = 0 if t < 32 else 1
                Y, bY = Y4[t % 2], bY4[t % 2]
                XMt, bXMt = xmt[t % 2], bxmt[t % 2]
                XO, bXO = xo[t % 2], bxo[t % 2]
                dve("tensor_scalar", R=[bY, bRT], W=[bacc], out=acc[:], in0=Y[:, 0, :], scalar1=W4[:, t, 0:1], scalar2=None, op0=ALU.mult)
                for k in range(1, 4):
                    dve("scalar_tensor_tensor", R=[bY, bRT, bacc], W=[bacc], out=acc[:], in0=Y[:, k, :], scalar=W4[:, t, k:k + 1], in1=acc[:],
                       op0=ALU.mult, op1=ALU.add)
                pool("tensor_tensor", R=[bacc, bMOD], W=[bacc], out=acc[:], in0=acc[:], in1=BC[:, w, 3, :], op=ALU.mult)
                dve("tensor_tensor", R=[bacc, bXMt], W=[bXO], out=XO[:], in0=acc[:], in1=XMt[:], op=ALU.add)
                if not last:
                    dma(XS[t * 128:(t + 1) * 128, :], XO[:], R=[bXO], W=[bXS], waw=False)
                else:
                    act("activation", R=[bXO], W=[bjunk, bc1], out=junk[:], in_=XO[:], func=AF.Square, accum_out=c1[:, 0:1])
                    act("activation", R=[bc1], W=[bc1], out=c1[:, 1:2], in_=c1[:, 0:1], func=AF.Sqrt, bias=EPS, scale=1.0 / D)
                    dve("reciprocal", R=[bc1], W=[bc1], out=c1[:, 2:3], in_=c1[:, 1:2])
                    dve("scalar_tensor_tensor", R=[bXO, bc1, bGF], W=[bXO], out=XO[:], in0=XO[:], scalar=c1[:, 2:3], in1=gfbc[:],
                        op0=ALU.mult, op1=ALU.mult)
                    dma(out_d[t * 128:(t + 1) * 128, :], XO[:], R=[bXO], W=[bOUT], waw=False)

        done = False
        for l in range(nlayers):
            compute_mod(l)
            with ExitStack() as lay:
                DEST = T(lay, [128, 34, 4], I32); W4 = T(lay, [128, 34, 4], F32); bRT = Buf("route")
                with ExitStack() as att:
                    KT = T(att, [128, NTOK], BF16); bKT = Buf("KT")
                    VA = T(att, [128, 34, 2, 128], BF16); bVA = Buf("VA")
                    with ExitStack() as ph:
                        phase1(l, ph, KT, VA, bKT, bVA)
                        fw.barrier()
                    if stop_after == (l, 1):
                        done = True
                    if not done:
                        with ExitStack() as ph:
                            phase2(l, ph, KT, VA, bKT, bVA, DEST, W4, bRT)
                            fw.barrier()
                        if stop_after == (l, 2):
                            done = True
                if not done:
                    with ExitStack() as ph:
                        phase3(l, ph)
                        fw.barrier()
                    if stop_after == (l, 3):
                        done = True
                if not done:
                    with ExitStack() as ph:
                        phase4(l, ph, DEST, W4, bRT)
                        fw.barrier()
            if done:
                break
        fw.barrier()
    return nc


def _rope_tables():
    pos = np.arange(S)
    r = (pos // 64).astype(np.float32)
    col = (pos % 64).astype(np.float32)
    inv = (10000.0 ** (-np.arange(0, 32, 2, dtype=np.float32) / 32.0)).astype(np.float32)
    ang = np.concatenate([r[:, None] * inv[None, :], col[:, None] * inv[None, :]], axis=-1).astype(np.float32)
    tab = np.concatenate([np.cos(ang), np.sin(ang)], axis=-1).astype(np.float32)
    return np.ascontiguousarray(tab.reshape(32, 128, 64).transpose(1, 0, 2))


def _col(v):
    v = np.asarray(v, dtype=np.float32)
    lead = v.shape[:-1]
    n = v.shape[-1] // 128
    a = v.reshape(*lead, n, 128)
    return np.ascontiguousarray(np.moveaxis(a, -1, 0))


def make_in_maps(inp, cores):
    f = lambda a: np.ascontiguousarray(np.asarray(a, dtype=np.float32))
    bc = lambda v: np.ascontiguousarray(np.broadcast_to(np.asarray(v, np.float32), (128,) + np.asarray(v).shape))
    shared = {
        "w_ada": f(inp["w_ada"]), "b_ada": f(inp["b_ada"]),
        "b_adac": _col(inp["b_ada"]),
        "g1c": _col(inp["g_norm1"]),
        "g2bc": np.ascontiguousarray(np.broadcast_to(f(inp["g_norm2"])[:, None, :], (2, 128, D))),
        "gfbc": bc(inp["g_final"]),
        "w_in": f(inp["w_in"]), "w_o": f(inp["w_o"]), "w_router": f(inp["w_router"]),
        "b_rbc": bc(inp["b_router"]),
        "gqk": bc(np.concatenate([np.tile(f(inp["g_q"]), (1, 8)), np.tile(f(inp["g_k"]), (1, 2))], axis=1)),
        "cs": _rope_tables(),
        "wdwc": np.ascontiguousarray(_col(inp["w_dw"])),
        "cvp": np.ascontiguousarray(np.stack([_col(inp["b_dw"]), _col(inp["g_conv_ln"]), _col(inp["b_conv_ln"])], axis=-1)),
        "sgp": bc(np.stack([f(inp["g_sgu_ln"]), f(inp["b_sgu_ln"])], axis=1)),
        "wsT": np.ascontiguousarray(f(inp["w_s"]).transpose(3, 0, 1, 2)),
        "bsc": np.ascontiguousarray(f(inp["b_s"]).transpose(2, 0, 1)),
        "w_gate": f(inp["w_gate"]), "w_up": f(inp["w_up"]), "w_down": f(inp["w_down"]),
        "bgu": np.ascontiguousarray(np.stack([_col(inp["b_gate"]), _col(inp["b_up"])], axis=3)),
        "b_down": f(inp["b_down"]),
        "ident": np.eye(128, dtype=np.float32),
        "tri": np.triu(np.ones((128, 128), np.float32), k=1),
        "eoff": bc(np.arange(NE, dtype=np.float32) * CAP),
        "iota": bc(np.arange(NE, dtype=np.float32)),
    }
    shared["wdwc"] = np.ascontiguousarray(shared["wdwc"].transpose(0, 1, 3, 2))
    maps = []
    for b in cores:
        m = dict(shared)
        m["x"] = f(inp["x"][b]); m["ctx"] = f(inp["ctx"][b])
        m["cc"] = np.ascontiguousarray(np.stack([_col(inp["c"][b]), _col(inp["c_ctx"])], axis=-1))
        maps.append(m)
    return maps


_NC = None


def kernel(**inputs):
    global _NC
    if _NC is None:
        _NC = build()
    maps = make_in_maps(inputs, list(range(8)))
    res = run_bass_kernel_spmd(_NC, maps, core_ids=list(range(8)))
    return np.stack([np.asarray(r["out"], dtype=np.float32) for r in res.results], axis=0)
```
